# Optimizing a Trainium2 kernel written in Bass

```python
import math
import jax, jax.numpy as jnp
from jax import lax
import numpy as np

D_MODEL = 1024
BATCH = 16
SEQ = 2048
DEPTH = 1

MEM_LEN = 256
EPS = 1e-6

SSD_HEADS = 16
SSD_HEAD_DIM = 64
SSD_INNER = SSD_HEADS * SSD_HEAD_DIM
SSD_GROUPS = 2
SSD_STATE = 128
SSD_CONV = 4
SSD_CHUNK = 128
SSD_CONV_DIM = SSD_INNER + 2 * SSD_GROUPS * SSD_STATE
DT_MIN = 0.001
DT_MAX = 0.1

GMLP_GROUPS = 8
GMLP_INNER = 1024
GMLP_GROUP_DIM = GMLP_INNER // GMLP_GROUPS
GMLP_CHUNK = 128

XA_HEADS = 4
XA_HEAD_DIM = 256
XA_INNER = XA_HEADS * XA_HEAD_DIM

N_BRANCH = 3
BRANCH_WIDTH = 1024

IN_DIM = SSD_INNER + SSD_CONV_DIM + SSD_HEADS + 2 * GMLP_INNER + XA_INNER + N_BRANCH * D_MODEL

PEER_HEADS = 8
PEER_N_KEYS = 128
PEER_N_EXPERTS = PEER_N_KEYS * PEER_N_KEYS
PEER_QUERY_DIM = 256
PEER_HALF = PEER_QUERY_DIM // 2
PEER_TOPK = 16
PEER_TOKEN_BLOCK = 128

kernel_name = 'hybrid_ssd_gmlp_memxattn_peer'


def rms_norm(x, g):
    xf = x.astype(jnp.float32)
    y = xf * lax.rsqrt(jnp.mean(xf * xf, axis=-1, keepdims=True) + EPS)
    return (y * g.astype(jnp.float32)).astype(x.dtype)


def causal_depthwise_conv(x, w, b):
    k = w.shape[0]
    y = lax.conv_general_dilated(
        x, w[:, None, :].astype(x.dtype), window_strides=(1,), padding=[(k - 1, 0)],
        dimension_numbers=('NWC', 'WIO', 'NWC'), feature_group_count=x.shape[-1])
    return y + b.astype(x.dtype)


def ssd_chunked(xh, dt, a, bm, cm):
    out_dtype = xh.dtype
    b, s = xh.shape[0], xh.shape[1]
    c, l = s // SSD_CHUNK, SSD_CHUNK
    g, r = SSD_GROUPS, SSD_HEADS // SSD_GROUPS
    xdt = (xh.astype(jnp.float32) * dt[..., None]).reshape(b, c, l, g, r, SSD_HEAD_DIM)
    adt = (dt * a).reshape(b, c, l, g, r).transpose(0, 3, 4, 1, 2)
    bc = bm.astype(jnp.float32).reshape(b, c, l, g, SSD_STATE)
    cc = cm.astype(jnp.float32).reshape(b, c, l, g, SSD_STATE)
    a_cs = jnp.cumsum(adt, axis=-1)
    causal = jnp.tril(jnp.ones((l, l), dtype=bool))
    seg = a_cs[..., :, None] - a_cs[..., None, :]
    lmat = jnp.exp(jnp.where(causal, seg, -jnp.inf))
    cb = jnp.einsum('bclgn,bcsgn->bcgls', cc, bc)
    y_diag = jnp.einsum('bcgls,bgrcls,bcsgrp->bclgrp', cb, lmat, xdt)
    decay = jnp.exp(a_cs[..., -1:] - a_cs)
    states = jnp.einsum('bclgn,bgrcl,bclgrp->cbgrpn', bc, decay, xdt)
    chunk_decay = jnp.exp(a_cs[..., -1]).transpose(3, 0, 1, 2)

    def step(carry, inp):
        st, dec = inp
        return carry * dec[..., None, None] + st, carry

    init = jnp.zeros(states.shape[1:], jnp.float32)
    _, prev = lax.scan(step, init, (states, chunk_decay))
    y_off = jnp.einsum('bclgn,cbgrpn,bgrcl->bclgrp', cc, prev, jnp.exp(a_cs))
    return (y_diag + y_off).reshape(b, s, SSD_HEADS, SSD_HEAD_DIM).astype(out_dtype)


def ssd_branch(xbc_raw, z, dt_raw, conv_w, conv_b, dt_bias, a_log, d_skip, ssd_norm):
    b, s = z.shape[0], z.shape[1]
    xbc = jax.nn.silu(causal_depthwise_conv(xbc_raw, conv_w, conv_b))
    xs, bm, cm = jnp.split(xbc, [SSD_INNER, SSD_INNER + SSD_GROUPS * SSD_STATE], axis=-1)
    xh = xs.reshape(b, s, SSD_HEADS, SSD_HEAD_DIM)
    dt = jax.nn.softplus(dt_raw.astype(jnp.float32) + dt_bias.astype(jnp.float32))
    a = -jnp.exp(a_log.astype(jnp.float32))
    y = ssd_chunked(xh, dt, a,
                    bm.reshape(b, s, SSD_GROUPS, SSD_STATE),
                    cm.reshape(b, s, SSD_GROUPS, SSD_STATE))
    y = (y + d_skip.astype(xh.dtype)[:, None] * xh).reshape(b, s, SSD_INNER)
    return rms_norm(y * jax.nn.silu(z), ssd_norm)


def gmlp_branch(u, v, gmlp_norm, w_spatial, b_spatial):
    b, s = u.shape[0], u.shape[1]
    u = jax.nn.gelu(u)
    v = rms_norm(jax.nn.gelu(v), gmlp_norm)
    vc = v.reshape(b, s // GMLP_CHUNK, GMLP_CHUNK, GMLP_GROUPS, GMLP_GROUP_DIM)
    w = w_spatial * jnp.tril(jnp.ones((GMLP_CHUNK, GMLP_CHUNK), w_spatial.dtype))
    mixed = jnp.einsum('gts,bcsgk->bctgk', w, vc) + b_spatial.T[:, :, None]
    return u * mixed.reshape(b, s, GMLP_INNER)


def memory_cross_attention(q, mem_n, w_mem_kv):
    b, s = q.shape[0], q.shape[1]
    m = mem_n.shape[1]
    k, v = jnp.split(mem_n @ w_mem_kv, 2, axis=-1)
    qh = q.reshape(b, s, XA_HEADS, XA_HEAD_DIM)
    kh = k.reshape(b, m, XA_HEADS, XA_HEAD_DIM)
    vh = v.reshape(b, m, XA_HEADS, XA_HEAD_DIM)
    scores = jnp.einsum('bshd,bmhd->bhsm', qh, kh).astype(jnp.float32) * (XA_HEAD_DIM ** -0.5)
    p = jax.nn.softmax(scores, axis=-1).astype(vh.dtype)
    return jnp.einsum('bhsm,bmhd->bshd', p, vh).reshape(b, s, XA_INNER)


def peer_ffn(xn, w_peer_q, peer_keys, peer_u, peer_v):
    b, s, d = xn.shape
    xb = xn.reshape((b * s) // PEER_TOKEN_BLOCK, PEER_TOKEN_BLOCK, d)

    def block(xt):
        t = xt.shape[0]
        q = (xt @ w_peer_q).reshape(t, PEER_HEADS, 2, PEER_HALF)
        sc = jnp.einsum('thid,hikd->thik', q, peer_keys).astype(jnp.float32)
        top_v, top_i = lax.top_k(sc, PEER_TOPK)
        cand = (top_v[:, :, 0, :, None] + top_v[:, :, 1, None, :]).reshape(t, PEER_HEADS, PEER_TOPK * PEER_TOPK)
        cidx = (top_i[:, :, 0, :, None] * PEER_N_KEYS + top_i[:, :, 1, None, :]).reshape(t, PEER_HEADS, PEER_TOPK * PEER_TOPK)
        best_v, best_pos = lax.top_k(cand, PEER_TOPK)
        eid = jnp.take_along_axis(cidx, best_pos, axis=-1)
        gate = jax.nn.softmax(best_v, axis=-1).astype(xt.dtype)
        u = peer_u[eid]
        act = jax.nn.gelu(jnp.einsum('thkd,td->thk', u, xt))
        v = peer_v[eid]
        return jnp.einsum('thk,thkd->td', gate * act, v)

    return lax.map(block, xb).reshape(b, s, d)


def setup_inputs(seed: int = 0) -> dict:
    key = jax.random.key(seed)
    ks = jax.random.split(key, 24)
    f32 = jnp.float32
    L = DEPTH

    def normal(k, shape, scale):
        return jax.random.normal(k, shape, f32) * scale

    def gain(k, shape):
        return 1.0 + 0.01 * jax.random.normal(k, shape, f32)

    x = normal(ks[0], (BATCH, SEQ, D_MODEL), 1.0)
    mem = normal(ks[1], (BATCH, MEM_LEN, D_MODEL), 1.0)
    ln_mix = gain(ks[2], (L, D_MODEL))
    w_in = normal(ks[3], (L, D_MODEL, IN_DIM), D_MODEL ** -0.5)
    conv_w = normal(ks[4], (L, SSD_CONV, SSD_CONV_DIM), SSD_CONV ** -0.5)
    conv_b = normal(ks[5], (L, SSD_CONV_DIM), 0.01)
    dt0 = jnp.exp(jax.random.uniform(ks[6], (L, SSD_HEADS), f32, math.log(DT_MIN), math.log(DT_MAX)))
    dt_bias = dt0 + jnp.log(-jnp.expm1(-dt0))
    a_log = jnp.log(jax.random.uniform(ks[7], (L, SSD_HEADS), f32, 1.0, 16.0))
    d_skip = gain(ks[8], (L, SSD_HEADS))
    ssd_norm = gain(ks[9], (L, SSD_INNER))
    gmlp_norm = gain(ks[10], (L, GMLP_INNER))
    w_spatial = normal(ks[11], (L, GMLP_GROUPS, GMLP_CHUNK, GMLP_CHUNK), GMLP_CHUNK ** -0.5)
    b_spatial = gain(ks[12], (L, GMLP_GROUPS, GMLP_CHUNK))
    ln_mem = gain(ks[13], (L, D_MODEL))
    w_mem_kv = normal(ks[14], (L, D_MODEL, 2 * XA_INNER), D_MODEL ** -0.5)
    w_branch = normal(ks[15], (L, N_BRANCH, BRANCH_WIDTH, D_MODEL), BRANCH_WIDTH ** -0.5)
    w_out = normal(ks[16], (L, D_MODEL, D_MODEL), D_MODEL ** -0.5)
    ln_ffn = gain(ks[17], (L, D_MODEL))
    w_peer_q = normal(ks[18], (L, D_MODEL, PEER_HEADS * PEER_QUERY_DIM), D_MODEL ** -0.5)
    peer_keys = normal(ks[19], (L, PEER_HEADS, 2, PEER_N_KEYS, PEER_HALF), PEER_HALF ** -0.5)
    peer_u = normal(ks[20], (L, PEER_N_EXPERTS, D_MODEL), D_MODEL ** -0.5)
    peer_v = normal(ks[21], (L, PEER_N_EXPERTS, D_MODEL), PEER_HEADS ** -0.5)
    ln_final = gain(ks[22], (D_MODEL,))
    return {'x': x, 'mem': mem, 'ln_mix': ln_mix, 'w_in': w_in, 'conv_w': conv_w, 'conv_b': conv_b,
            'dt_bias': dt_bias, 'a_log': a_log, 'd_skip': d_skip, 'ssd_norm': ssd_norm,
            'gmlp_norm': gmlp_norm, 'w_spatial': w_spatial, 'b_spatial': b_spatial,
            'ln_mem': ln_mem, 'w_mem_kv': w_mem_kv, 'w_branch': w_branch, 'w_out': w_out,
            'ln_ffn': ln_ffn, 'w_peer_q': w_peer_q, 'peer_keys': peer_keys, 'peer_u': peer_u,
            'peer_v': peer_v, 'ln_final': ln_final}


def reference(x, mem, ln_mix, w_in, conv_w, conv_b, dt_bias, a_log, d_skip, ssd_norm,
              gmlp_norm, w_spatial, b_spatial, ln_mem, w_mem_kv, w_branch, w_out,
              ln_ffn, w_peer_q, peer_keys, peer_u, peer_v, ln_final):
    b, s = x.shape[0], x.shape[1]
    bounds = np.cumsum([SSD_INNER, SSD_CONV_DIM, SSD_HEADS, GMLP_INNER, GMLP_INNER, XA_INNER]).tolist()
    for l in range(DEPTH):
        h = rms_norm(x, ln_mix[l])
        proj = h @ w_in[l]
        z, xbc, dt_raw, u, v, q, gates = jnp.split(proj, bounds, axis=-1)
        y_ssd = ssd_branch(xbc, z, dt_raw, conv_w[l], conv_b[l], dt_bias[l], a_log[l], d_skip[l], ssd_norm[l])
        y_gmlp = gmlp_branch(u, v, gmlp_norm[l], w_spatial[l], b_spatial[l])
        y_mem = memory_cross_attention(q, rms_norm(mem, ln_mem[l]), w_mem_kv[l])
        branches = jnp.stack([y_ssd, y_gmlp, y_mem], axis=2)
        proj_b = jnp.einsum('bsnw,nwd->bsnd', branches, w_branch[l])
        g = jax.nn.sigmoid(gates.reshape(b, s, N_BRANCH, D_MODEL))
        merged = jnp.sum(g * proj_b, axis=2)
        x = x + merged @ w_out[l]
        x = x + peer_ffn(rms_norm(x, ln_ffn[l]), w_peer_q[l], peer_keys[l], peer_u[l], peer_v[l])
    return rms_norm(x, ln_final)
```

```python
import numpy as np
import concourse.bass as bass
import concourse.mybir as mybir
from concourse.bass_utils import run_bass_kernel_spmd

F32 = mybir.dt.float32
BF16 = mybir.dt.bfloat16
U32 = mybir.dt.uint32
ALU = mybir.AluOpType
AF = mybir.ActivationFunctionType
AX = mybir.AxisListType

SB_LO = 16512
SB_HI = 229344
NDMA_SEM = 32
ENGS = ("pe", "dve", "act", "pool", "sp")

NCORES = 8
TOK = 4096
NCH = 32
D = 1024
IN_DIM = 8720
EPS = 1e-6
O_Z, O_XBC, O_DT, O_U, O_V, O_Q, O_G = 0, 1024, 2560, 2576, 3600, 4624, 5648
C_ID, C_TRIU, C_ONES, C_S, C_P, C_NEG, C_IOTA, C_R0, NC_CONST = 0, 128, 256, 384, 768, 1152, 1280, 1408, 1536
V_LNMIX, V_CONVW, V_CONVB, V_DTB, V_ALOG, V_DSKIP, V_SSDN, V_GMLPN, V_LNMEM, V_LNFFN, V_LNFIN, NV = (
    0, 1024, 7168, 8704, 8720, 8736, 8752, 9776, 10800, 11824, 12848, 13872)


class Buf:
    __slots__ = ("name", "writers", "readers", "t")

    def __init__(self, name, t=None):
        self.name = name
        self.writers = {}
        self.readers = {}
        self.t = t

    def sub(self, name):
        return Buf(self.name + "." + name, self.t)

    def __getitem__(self, key):
        return self.t[key]

    def ap(self):
        return self.t.ap()


class Op:
    __slots__ = ("eng", "fn", "deps", "is_dma", "signal", "sem", "val", "gidx")

    def __init__(self, eng, fn, is_dma, gidx):
        self.eng = eng
        self.fn = fn
        self.is_dma = is_dma
        self.deps = []
        self.signal = False
        self.sem = None
        self.val = 0
        self.gidx = gidx


class K:
    def __init__(self, nc):
        self.nc = nc
        self.ops = {e: [] for e in ENGS}
        self.all_ops = []
        self.sb_ptr = SB_LO
        self.marks = []
        self.n_dma = 0
        self.n_dma_sw = 0
        self.dma_last = [None] * NDMA_SEM
        self.dma_cnt = [0] * NDMA_SEM
        self.dma_sems = [nc.alloc_semaphore("dsem%d" % i) for i in range(NDMA_SEM)]
        self.eng_sems = {e: nc.alloc_semaphore("esem_" + e) for e in ENGS}
        self.pending_dma = []
        self.nameid = 0
        self.sb_hi = SB_HI

    def sb_top(self, name, shape, dtype):
        esz = {F32: 4, BF16: 2, U32: 4}[dtype]
        n = 1
        for s in shape[1:]:
            n *= s
        nbytes = (n * esz + 31) // 32 * 32
        self.sb_hi -= nbytes
        assert self.sb_hi >= self.sb_ptr
        self.nameid += 1
        t = self.nc.alloc_sbuf_tensor_at("%s_%d" % (name, self.nameid), list(shape), dtype, offset=self.sb_hi)
        return Buf(name, t)

    def sb(self, name, shape, dtype):
        esz = {F32: 4, BF16: 2, U32: 4}[dtype]
        n = 1
        for s in shape[1:]:
            n *= s
        nbytes = (n * esz + 31) // 32 * 32
        off = self.sb_ptr
        assert off + nbytes <= self.sb_hi, "SBUF overflow at %s: need %d have %d" % (name, nbytes, self.sb_hi - off)
        self.sb_ptr += nbytes
        self.nameid += 1
        t = self.nc.alloc_sbuf_tensor_at("%s_%d" % (name, self.nameid), list(shape), dtype, offset=off)
        return Buf(name, t)

    def mark(self):
        self.marks.append(self.sb_ptr)

    def release(self):
        self.sb_ptr = self.marks.pop()

    def op(self, eng, fn, reads=(), writes=(), dma=False):
        o = Op(eng, fn, dma, len(self.all_ops))
        deps = {}
        for b in reads:
            for w in b.writers.values():
                deps[w] = True
        for b in writes:
            for w in b.writers.values():
                deps.setdefault(w, False)
            for r in b.readers.values():
                deps.setdefault(r, False)
        if dma:
            if eng == "pool":
                si = NDMA_SEM - 8 + self.n_dma_sw % 8
                self.n_dma_sw += 1
            else:
                si = self.n_dma % (NDMA_SEM - 8)
                self.n_dma += 1
            prev = self.dma_last[si]
            if prev is not None:
                deps.setdefault(prev, True)
            self.dma_last[si] = o
            self.dma_cnt[si] += 1
            o.sem = self.dma_sems[si]
            o.val = 16 * self.dma_cnt[si]
            o.signal = True
            self.pending_dma.append(o)
        for d, raw in deps.items():
            if d is o:
                continue
            if (not d.is_dma) and (not dma) and d.eng == eng:
                if eng == "pe":
                    continue
            o.deps.append(d)
        key = ("dma", o.gidx) if dma else eng
        for b in reads:
            b.readers[key] = o
        for b in writes:
            b.writers = {key: o}
            b.readers = {}
        self.ops[eng].append(o)
        self.all_ops.append(o)
        return o

    def barrier(self):
        lasts = []
        for e in ENGS:
            for o in reversed(self.ops[e]):
                if (not o.is_dma) and o.fn is not None:
                    lasts.append(o)
                    break
        pend = list(self.pending_dma)
        self.pending_dma = []
        for e in ENGS:
            o = Op(e, None, False, len(self.all_ops))
            for d in lasts + pend:
                if d.eng == e and not d.is_dma:
                    continue
                o.deps.append(d)
            self.ops[e].append(o)
            self.all_ops.append(o)

    def emit(self):
        nc = self.nc
        for o in self.all_ops:
            for d in o.deps:
                d.signal = True
        for e in ENGS:
            c = 0
            for o in self.ops[e]:
                if o.is_dma:
                    continue
                if o.signal:
                    c += 1
                    o.sem = self.eng_sems[e]
                    o.val = c

        def run(engname):
            def body(e):
                waited = {}
                for o in self.ops[engname]:
                    for d in o.deps:
                        s, v = d.sem, d.val
                        if waited.get(s.num, 0) >= v:
                            continue
                        e.wait_ge(s, v)
                        waited[s.num] = v
                    if o.fn is None:
                        continue
                    ins = o.fn(e)
                    if o.signal:
                        ins.then_inc(o.sem, 16 if o.is_dma else 1)
            return body

        with nc.Block() as block:
            block.sync(run("sp"))
            block.scalar(run("act"))
            block.tensor(run("pe"))
            block.vector(run("dve"))
            block.gpsimd(run("pool"))


def build_nc(n_ch=NCH, stop_after=None, debug=False):
    nc = bass.Bass("TRN2", target_bir_lowering=False)
    k = K(nc)
    ntok = n_ch * 128

    def din(name, shape, dt=F32):
        return nc.dram_tensor(name, list(shape), dt, kind="ExternalInput")

    x_d = din("x", [TOK, D]); mem_d = din("mem", [512, D])
    win_d = din("w_in", [D, IN_DIM]); wkv_d = din("w_kv", [D, 2048]); wbr_d = din("w_br", [3 * D, D])
    wout_d = din("w_out", [D, D]); wpq_d = din("w_pq", [D, 2048]); keys_d = din("keysT", [128, 2048])
    wsp_d = din("wspT", [128, 1024]); uh_d = din("Uh", [16384, D]); v_d = din("V", [16384, D])
    vecs_d = din("vecs", [128, NV]); consts_d = din("consts", [128, NC_CONST]); bsp_d = din("bsp", [128, 8])
    out_d = nc.dram_tensor("out", [TOK, D], F32, kind="ExternalOutput")
    proj_d = nc.dram_tensor("proj_s", [TOK, IN_DIM], BF16, kind="Internal")
    dtraw_d = nc.dram_tensor("dtraw_s", [TOK, 16], F32, kind="Internal")
    x1_d = nc.dram_tensor("x1_s", [TOK, D], F32, kind="Internal")
    ub_d = nc.dram_tensor("ub_s", [16384, D], BF16, kind="Internal")
    vb_d = nc.dram_tensor("vb_s", [16384, D], BF16, kind="Internal")
    xng_d = nc.dram_tensor("xng_s", [NCH // 2, 128, 8, 256], BF16, kind="Internal")
    wbrb_d = nc.dram_tensor("wbrb_s", [3 * D, D], BF16, kind="Internal")
    woutb_d = nc.dram_tensor("woutb_s", [D, D], BF16, kind="Internal")
    wpqb_d = nc.dram_tensor("wpqb_s", [D, 2048], BF16, kind="Internal")
    dbg = {}
    if debug:
        for nm, shp in debug.items():
            dbg[nm] = nc.dram_tensor("dbg_" + nm, list(shp), F32, kind="ExternalOutput")

    B_proj = [Buf("proj%d" % c) for c in range(NCH)]
    B_dtraw = [Buf("dtraw%d" % c) for c in range(NCH)]
    B_x1 = [Buf("x1_%d" % c) for c in range(NCH)]
    B_ub = [Buf("ub%d" % i) for i in range(16)]
    B_vb = [Buf("vb%d" % i) for i in range(16)]
    B_out = Buf("out")
    B_wbrb = [Buf("wbrb%d" % i) for i in range(3)]
    B_woutb = Buf("woutb")
    B_wpqb = Buf("wpqb")
    B_xng = [Buf("xng%d" % c) for c in range(NCH)]
    B_in = Buf("inputs")

    ps = [Buf("ps%d" % i, nc.alloc_psum_tensor("ps%d" % i, [128, 512], F32)) for i in range(8)]
    psb = [p.t.bitcast(BF16) for p in ps]

    def mm(pb, pout, lb, lap, rb, rap, st=True, sp=True):
        k.op("pe", lambda e: e.matmul(pout, lap, rap, start=st, stop=sp), reads=[lb, rb], writes=[pb])

    def trn(pb, pout, ib, iap, idap):
        k.op("pe", lambda e: e.transpose(pout, iap, idap), reads=[ib, cbf], writes=[pb])

    def act(ob, oap, ib, iap, func, bias=None, scale=1.0, accum=None, extra=(), wextra=()):
        kw = {}
        if bias is not None:
            kw["bias"] = bias
        if accum is not None:
            kw["accum_out"] = accum
        k.op("act", lambda e: e.activation(out=oap, in_=iap, func=func, scale=scale, **kw),
             reads=[ib] + list(extra), writes=[ob] + list(wextra))

    def tt(eng, ob, oap, ab, aap, bb, bap, op):
        k.op(eng, lambda e: e.tensor_tensor(out=oap, in0=aap, in1=bap, op=op), reads=[ab, bb], writes=[ob])

    def ts(eng, ob, oap, ab, aap, s1, s2, op0, op1=ALU.bypass, extra=()):
        if s2 is None:
            k.op(eng, lambda e: e.tensor_scalar(out=oap, in0=aap, scalar1=s1, scalar2=None, op0=op0),
                 reads=[ab] + list(extra), writes=[ob])
        else:
            k.op(eng, lambda e: e.tensor_scalar(out=oap, in0=aap, scalar1=s1, scalar2=s2, op0=op0, op1=op1),
                 reads=[ab] + list(extra), writes=[ob])

    def stt(ob, oap, ab, aap, scalar, bb, bap, op0, op1, extra=()):
        k.op("dve", lambda e: e.scalar_tensor_tensor(out=oap, in0=aap, scalar=scalar, in1=bap, op0=op0, op1=op1),
             reads=[ab, bb] + list(extra), writes=[ob])

    def cp(eng, ob, oap, ib, iap):
        if eng == "act":
            k.op("act", lambda e: e.activation(out=oap, in_=iap, func=AF.Copy), reads=[ib], writes=[ob])
        else:
            k.op(eng, lambda e: e.tensor_copy(out=oap, in_=iap), reads=[ib], writes=[ob])

    def dma(q, ob, oap, ib, iap, nobar=False, **kw):
        o = k.op(q, lambda e: e.dma_start(out=oap, in_=iap, **kw), reads=[ib], writes=[ob], dma=True)
        if nobar:
            k.pending_dma.remove(o)

    def dump(name, b, ap_, shape):
        if name in dbg:
            k.mark()
            tmp = k.sb("dbgtmp", list(shape), F32)
            cp("dve", tmp, tmp.ap(), b, ap_)
            dma("sp", B_out, dbg[name].ap(), tmp, tmp.ap())
            k.op("sp", None, reads=[B_out])
            k.barrier()
            k.release()

    cf = k.sb("cf", [128, 384 + 16], F32)
    cbf = k.sb("cbf", [128, NC_CONST], BF16)
    dma("sp", cf, cf[:, 0:384], B_in, consts_d[:, 0:384])
    dma("sp", cf, cf[:, 384:400], B_in, consts_d[:, C_IOTA:C_IOTA + 16])
    dma("pool", cbf, cbf.ap(), B_in, consts_d.ap(), max_dma_last_dim=4096)
    idb = cbf[:, C_ID:C_ID + 128]
    idf = cf[:, C_ID:C_ID + 128]
    small = k.sb("small", [128, 64], F32)
    junk = k.sb("junk", [128, 1024], BF16)

    k.op("dve", lambda e: e.memset(small[:, 8:9], -0.5), writes=[small])

    def rmsnorm(xb, xap, gb, gap, ob, oap, tag, pool_pow=False):
        act(junk, junk.ap(), xb, xap, AF.Square, accum=small[:, 0:1], wextra=[small])
        ts("dve", small, small[:, 1:2], small, small[:, 0:1], 1.0 / D, EPS, ALU.mult, ALU.add)
        if pool_pow:
            k.op("pool", lambda e: e.tensor_tensor(out=small[:, 3:4], in0=small[:, 1:2], in1=small[:, 8:9], op=ALU.pow),
                 reads=[small], writes=[small])
        else:
            act(small, small[:, 2:3], small, small[:, 1:2], AF.Sqrt)
            k.op("dve", lambda e: e.reciprocal(out=small[:, 3:4], in_=small[:, 2:3]), reads=[small], writes=[small])
        stt(ob, oap, xb, xap, small[:, 3:4], gb, gap, ALU.mult, ALU.mult, extra=[small])

    KT = k.sb_top("KT", [128, 2, 8, 256], BF16)
    Vm = k.sb_top("Vm", [128, 2, 2, 1024], BF16)


    k.mark()
    win = k.sb("win", [128, 8, IN_DIM], BF16)
    win_nb = [win.sub("c%d" % nb) for nb in range(18)]
    k.mark()
    wkv = k.sb("wkv", [128, 8, 2048], BF16)
    lnmem = k.sb("lnmem", [128, 1024], F32)
    memt = k.sb("memt", [128, 1024], F32)
    memn = k.sb("memn", [128, 1024], BF16)
    memT = k.sb("memT", [128, 8, 128], BF16)
    for kc in range(8):
        dma("pool", wkv, wkv[:, kc, :], B_in, wkv_d[kc * 128:(kc + 1) * 128, :], max_dma_last_dim=4096)
    for nb in range(18):
        c0_ = nb * 512
        w_ = min(512, IN_DIM - c0_)
        dma("pool", win_nb[nb], win[:, :, c0_:c0_ + w_], B_in, win_d[:, c0_:c0_ + w_].rearrange("(kc p) n -> p kc n", p=128),
            nobar=True, max_dma_last_dim=4096)
    dma("sp", lnmem, lnmem.ap(), B_in, vecs_d[:, V_LNMEM:V_LNMEM + 1024])
    for mt in range(4):
        b, j = mt // 2, mt % 2
        dma("sp", memt, memt.ap(), B_in, mem_d[mt * 128:(mt + 1) * 128, :])
        rmsnorm(memt, memt.ap(), lnmem, lnmem.ap(), memn, memn.ap(), "mem")
        for kc in range(8):
            trn(ps[0], psb[0][:, kc * 128:(kc + 1) * 128], memn, memn[:, kc * 128:(kc + 1) * 128], idb)
        cp("dve", memT, memT.ap().rearrange("p a b -> p (a b)"), ps[0], psb[0][:, :])
        for dc in range(8):
            pb = ps[1 + dc // 4]
            for kc in range(8):
                mm(pb, pb[:, (dc % 4) * 128:(dc % 4 + 1) * 128], wkv, wkv[:, kc, dc * 128:(dc + 1) * 128],
                   memT, memT[:, kc, :], st=(kc == 0), sp=(kc == 7))
        for h2 in range(2):
            pb = ps[1 + h2]
            cp("act", KT, KT[:, b, h2 * 4:(h2 + 1) * 4, j * 128:(j + 1) * 128],
               pb, pb.ap().rearrange("p (a b) -> p a b", a=4))
        for h2 in range(2):
            pb = ps[3 + h2]
            for kc in range(8):
                mm(pb, pb.ap(), memT, memT[:, kc, :], wkv, wkv[:, kc, 1024 + h2 * 512:1024 + (h2 + 1) * 512],
                   st=(kc == 0), sp=(kc == 7))
            cp("dve", Vm, Vm[:, b, j, h2 * 512:(h2 + 1) * 512], pb, pb.ap())
    k.barrier()
    k.release()
    if stop_after == 0:
        dump("KT", KT, KT.ap().rearrange("p a b c -> p (a b c)"), [128, 4096])
        dump("Vm", Vm, Vm.ap().rearrange("p a b c -> p (a b c)"), [128, 4096])
        k.emit()
        return nc

    k.mark()
    lnmix = k.sb("lnmix", [128, 1024], F32)
    xts = [k.sb("xt%d" % i, [128, 1024], F32) for i in range(1)]
    hb = k.sb("hb", [128, 1024], BF16)
    hTs = [k.sb("hT%d" % i, [128, 8, 128], BF16) for i in range(1)]
    stg = [k.sb("stg%d" % i, [128, IN_DIM], BF16) for i in range(2)]
    stg_sub = [[stg[i].sub("b%d" % nb) for nb in range(18)] for i in range(2)]
    dts = [k.sb("dts%d" % i, [128, 16], F32) for i in range(2)]
    dma("sp", lnmix, lnmix.ap(), B_in, vecs_d[:, V_LNMIX:V_LNMIX + 1024])
    for i in range(3):
        dma("pool", B_wbrb[i], wbrb_d[i * 1024:(i + 1) * 1024, :], B_in, wbr_d[i * 1024:(i + 1) * 1024, :], max_dma_last_dim=4096)
    dma("pool", B_woutb, woutb_d.ap(), B_in, wout_d.ap(), max_dma_last_dim=4096)
    dma("pool", B_wpqb, wpqb_d.ap(), B_in, wpq_d.ap(), max_dma_last_dim=4096)
    for i in range(16):
        dma("pool", B_ub[i], ub_d[i * 1024:(i + 1) * 1024, :], B_in, uh_d[i * 1024:(i + 1) * 1024, :], max_dma_last_dim=4096)
        dma("pool", B_vb[i], vb_d[i * 1024:(i + 1) * 1024, :], B_in, v_d[i * 1024:(i + 1) * 1024, :], max_dma_last_dim=4096)
    NBLK = 18
    for c in range(n_ch):
        par = c % 2
        xt, hT, st_, dt_ = xts[0], hTs[0], stg[par], dts[par]
        if c == 0:
            dma("sp", xt, xt.ap(), B_in, x_d[0:128, :])
        rmsnorm(xt, xt.ap(), lnmix, lnmix.ap(), hb, hb.ap(), "mix")
        if c + 1 < n_ch:
            dma("sp", xt, xt.ap(), B_in, x_d[(c + 1) * 128:(c + 2) * 128, :])
        for kc in range(8):
            trn(ps[0], psb[0][:, kc * 128:(kc + 1) * 128], hb, hb[:, kc * 128:(kc + 1) * 128], idb)
        cp("dve", hT, hT.ap().rearrange("p a b -> p (a b)"), ps[0], psb[0][:, :])
        subs = []
        for nb in range(NBLK):
            c0 = nb * 512
            w = min(512, IN_DIM - c0)
            pb = ps[1 + nb % 7]
            for kc in range(8):
                mm(pb, pb[:, 0:w], hT, hT[:, kc, :], win_nb[nb], win[:, kc, c0:c0 + w], st=(kc == 0), sp=(kc == 7))
            sbuf_ = stg_sub[par][nb]
            subs.append(sbuf_)
            cp("act" if nb % 2 == 0 else "dve", sbuf_, st_[:, c0:c0 + w], pb, pb[:, 0:w])
            if nb == 5:
                cp("dve", dt_, dt_.ap(), pb, pb[:, 0:16])
        k.op("sp", lambda e, c=c, st_=st_: e.dma_start(out=proj_d[c * 128:(c + 1) * 128, :], in_=st_.ap()),
             reads=subs, writes=[B_proj[c]], dma=True)
        dma("sp", B_dtraw[c], dtraw_d[c * 128:(c + 1) * 128, :], dt_, dt_.ap())
    k.barrier()
    k.release()
    k.release()
    if stop_after == 1:
        k.emit()
        return nc

    k.mark()
    wbr = k.sb("wbr", [128, 24, 1024], BF16)
    wout = k.sb("wout", [128, 8, 1024], BF16)
    wsp = k.sb("wsp", [128, 8, 128], BF16)
    convw = k.sb("convw", [128, 4, 1536], BF16)
    convb = k.sb("convb", [128, 1536], BF16)
    vsm = k.sb("vsm", [128, 48], F32)
    aneg = k.sb("aneg", [128, 16], F32)
    ssdn = k.sb("ssdn", [128, 1024], F32)
    gmlpn = k.sb("gmlpn", [128, 1024], F32)
    bsp = k.sb("bsp", [128, 8], F32)
    for i in range(3):
        dma("sp", wbr, wbr[:, i * 8:(i + 1) * 8, :], B_wbrb[i], wbrb_d[i * 1024:(i + 1) * 1024, :].rearrange("(kc p) n -> p kc n", p=128))
    dma("sp", wout, wout.ap(), B_woutb, woutb_d.ap().rearrange("(kc p) n -> p kc n", p=128))
    k.mark()
    wspf = k.sb("wspf", [128, 8, 128], F32)
    dma("sp", wspf, wspf.ap().rearrange("p a b -> p (a b)"), B_in, wsp_d.ap())
    tt("dve", wsp, wsp.ap(), wspf, wspf.ap(), cf, cf[:, C_TRIU:C_TRIU + 128].unsqueeze(1).to_broadcast([128, 8, 128]), ALU.mult)
    dma("pool", convw, convw.ap().rearrange("p a b -> p (a b)"), B_in, vecs_d[:, V_CONVW:V_CONVW + 6144], max_dma_last_dim=4096)
    dma("pool", convb, convb.ap(), B_in, vecs_d[:, V_CONVB:V_CONVB + 1536], max_dma_last_dim=4096)
    dma("sp", vsm, vsm.ap(), B_in, vecs_d[:, V_DTB:V_DTB + 48])
    dma("sp", ssdn, ssdn.ap(), B_in, vecs_d[:, V_SSDN:V_SSDN + 1024])
    dma("sp", gmlpn, gmlpn.ap(), B_in, vecs_d[:, V_GMLPN:V_GMLPN + 1024])
    dma("sp", bsp, bsp.ap(), B_in, bsp_d.ap())
    act(aneg, aneg.ap(), vsm, vsm[:, 16:32], AF.Exp)
    ts("dve", aneg, aneg.ap(), aneg, aneg.ap(), -1.0, None, ALU.mult)
    k.barrier()
    k.release()
    dtb = vsm[:, 0:16]
    dsk = vsm[:, 32:48]

    pj = k.sb("pj", [128, IN_DIM], BF16)
    xbcs = [k.sb("xbc%d" % i, [128, 1536], BF16) for i in range(2)]
    dtall = k.sb("dtall", [128, NCH, 16], F32)
    xt = k.sb("xt2", [128, 1024], F32)
    carry = k.sb("carry", [128, 1536], BF16)
    xsk = k.sb("xsk", [128, 4, 1536], BF16)
    xa = k.sb("xa", [128, 1536], BF16)
    BCT = k.sb("BCT", [128, 4, 128], BF16)
    s16 = k.sb("s16", [128, 8, 16], F32)
    acs2 = k.sb("acs2", [128, 32], F32)
    rAs = [k.sb("rA%d" % i, [128, 4, 128], F32) for i in range(2)]
    LT = k.sb("LT", [128, 16, 128], BF16)
    Mh = LT
    xdt = k.sb("xdt", [128, 1024], BF16)
    xdtd = k.sb("xdtd", [128, 1024], BF16)
    xsD = k.sb("xsD", [128, 1024], BF16)
    yf = k.sb("yf", [128, 1024], F32)
    t1 = k.sb("t1", [128, 1024], F32)
    stf = k.sb("stf", [128, 1024], F32)
    stb = k.sb("stb", [128, 1024], BF16)
    ybr = k.sb("ybr", [128, 1024], BF16)
    bT = k.sb("bT", [128, 8, 128], BF16)
    sg = k.sb("sg", [128, 1024], BF16)
    mg = k.sb("mg", [128, 1024], F32)
    sm8 = k.sb("sm8", [128, 16], F32)
    gu = xsD
    pexp = k.sb("pexp", [128, 1024], BF16)
    pexp3 = pexp.ap().rearrange("p (a b) -> p a b", a=4)

    def s16v(i):
        return s16[:, i, :]

    def transpose8(src_b, src_ap_fn, dst):
        for kc in range(8):
            trn(ps[7], psb[7][:, kc * 128:(kc + 1) * 128], src_b, src_ap_fn(kc), idb)
        cp("act", dst, dst.ap().rearrange("p a b -> p (a b)"), ps[7], psb[7][:, :])

    sg2 = k.sb("sg2", [128, 1024], BF16)
    bT2 = k.sb("bT2", [128, 8, 128], BF16)
    ybr2 = k.sb("ybr2", [128, 1024], BF16)

    def branch_proj(br, first, sg=sg, pre=False, src=None):
        src = ybr if src is None else src
        if not pre:
            act(sg, sg.ap(), pj, pj[:, O_G + br * 1024:O_G + (br + 1) * 1024], AF.Tanh, scale=0.5)
        transpose8(src, lambda kc: src[:, kc * 128:(kc + 1) * 128], bT)
        for h2 in range(2):
            pb = ps[5 + h2]
            sl = slice(h2 * 512, (h2 + 1) * 512)
            for kc in range(8):
                mm(pb, pb.ap(), bT, bT[:, kc, :], wbr, wbr[:, br * 8 + kc, sl], st=(kc == 0), sp=(kc == 7))
            if first:
                stt(mg, mg[:, sl], sg, sg[:, sl], 1.0, pb, pb.ap(), ALU.add, ALU.mult)
            else:
                stt(t1, t1[:, sl], sg, sg[:, sl], 1.0, pb, pb.ap(), ALU.add, ALU.mult)
                tt("pool", mg, mg[:, sl], mg, mg[:, sl], t1, t1[:, sl], ALU.add)

    k.op("sp", lambda e: e.dma_start(out=dtall[:, 0:n_ch, :], in_=dtraw_d[0:n_ch * 128, :].rearrange("(c p) h -> p c h", p=128)),
         reads=B_dtraw[0:n_ch], writes=[dtall], dma=True)
    tt("dve", dtall, dtall[:, 0:n_ch, :], dtall, dtall[:, 0:n_ch, :], vsm, dtb.unsqueeze(1).to_broadcast([128, n_ch, 16]), ALU.add)
    act(dtall, dtall[:, 0:n_ch, :], dtall, dtall[:, 0:n_ch, :], AF.Exp)
    act(dtall, dtall[:, 0:n_ch, :], dtall, dtall[:, 0:n_ch, :], AF.Ln, bias=1.0)

    def load_xbc(c):
        dma("sp", xbcs[c % 2], xbcs[c % 2].ap(), B_proj[c], proj_d[c * 128:(c + 1) * 128, O_XBC:O_XBC + 1536])

    xs3v = xa[:, 0:1024].rearrange("p (h q) -> p h q", h=16)

    def bc16(i):
        return s16v(i).unsqueeze(2).to_broadcast([128, 16, 64])

    def ssd_front(c):
        b, ci = c // 16, c % 16
        xbc = xbcs[c % 2]
        for kk in range(4):
            tt("dve" if kk < 3 else "pool", xsk, xsk[:, kk, :], xbc, xbc.ap(), convw, convw[:, kk, :], ALU.mult)
        yield
        for nb in range(3):
            pb = ps[nb]
            cs = slice(nb * 512, (nb + 1) * 512)
            mm(pb, pb.ap(), cbf, cbf[:, C_R0:C_R0 + 128], convb, convb[:, cs], st=True, sp=False)
            for kk in range(3):
                mm(pb, pb.ap(), cbf, cbf[:, C_S + kk * 128:C_S + (kk + 1) * 128], xsk, xsk[:, kk, cs], st=False, sp=False)
            if ci > 0:
                mm(pb, pb.ap(), cbf, idb, carry, carry[:, cs], st=False, sp=False)
            mm(pb, pb.ap(), cbf, idb, xsk, xsk[:, 3, cs], st=False, sp=True)
            if nb == 1:
                yield
        yield
        if ci < 15:
            for nb in range(3):
                pb = ps[3 + nb % 2]
                cs = slice(nb * 512, (nb + 1) * 512)
                for kk in range(3):
                    mm(pb, pb.ap(), cbf, cbf[:, C_P + kk * 128:C_P + (kk + 1) * 128], xsk, xsk[:, kk, cs], st=(kk == 0), sp=(kk == 2))
                cp("act", carry, carry[:, cs], pb, pb.ap())
        for nb in range(3):
            act(xa, xa[:, nb * 512:(nb + 1) * 512], ps[nb], ps[nb].ap(), AF.Silu)
        cp("dve", s16, s16v(2), dtall, dtall[:, c, :])
        tt("dve", s16, s16v(3), dtall, dtall[:, c, :], aneg, aneg.ap(), ALU.mult)
        yield
        for q in range(4):
            trn(ps[3], psb[3][:, q * 128:(q + 1) * 128], xa, xa[:, 1024 + q * 128:1024 + (q + 1) * 128], idb)
        cp("dve", BCT, BCT.ap().rearrange("p a b -> p (a b)"), ps[3], psb[3][:, 0:512])
        mm(ps[4], ps[4][:, 0:16], cf, cf[:, C_TRIU:C_TRIU + 128], s16, s16v(3))
        mm(ps[4], ps[4][:, 16:32], cf, cf[:, C_ONES:C_ONES + 128], s16, s16v(3))
        cp("dve", acs2, acs2.ap(), ps[4], ps[4][:, 0:32])
        ts("dve", s16, s16v(4), acs2, acs2[:, 0:16], -1.0, None, ALU.mult)
        act(s16, s16v(5), acs2, acs2[:, 0:16], AF.Exp)
        tt("dve", s16, s16v(6), acs2, acs2[:, 16:32], acs2, acs2[:, 0:16], ALU.subtract)
        act(s16, s16v(6), s16, s16v(6), AF.Exp)
        act(s16, s16v(7), acs2, acs2[:, 16:32], AF.Exp)
        for q in range(4):
            rA = rAs[q % 2]
            tt("pool", rA, rA.ap(), cf, cf[:, C_TRIU:C_TRIU + 128].unsqueeze(1).to_broadcast([128, 4, 128]),
               s16, s16[:, 3, 4 * q:4 * q + 4].unsqueeze(2).to_broadcast([128, 4, 128]), ALU.mult)
            pb = ps[q]
            mm(pb, pb.ap(), cf, cf[:, C_ONES:C_ONES + 128], rA, rA.ap().rearrange("p a b -> p (a b)"), st=True, sp=False)
            for hh in range(4):
                mm(pb, pb[:, hh * 128:(hh + 1) * 128], cbf, idb, cbf, cbf[:, C_NEG:C_NEG + 128], st=False, sp=(hh == 3))
            for hh in range(4):
                h = 4 * q + hh
                act(LT, LT[:, h, :], pb, pb[:, hh * 128:(hh + 1) * 128], AF.Exp, bias=s16[:, 4, h:h + 1], extra=[s16])
        for g in range(2):
            mm(ps[4], ps[4][:, g * 128:(g + 1) * 128], BCT, BCT[:, g, :], BCT, BCT[:, 2 + g, :])
        for g in range(2):
            tt("dve", Mh, Mh[:, g * 8:(g + 1) * 8, :], ps[4], ps[4][:, g * 128:(g + 1) * 128].unsqueeze(1).to_broadcast([128, 8, 128]),
               LT, LT[:, g * 8:(g + 1) * 8, :], ALU.mult)
        yield
        tt("dve", xdt, xdt.ap().rearrange("p (h q) -> p h q", h=16), xa, xs3v, s16, bc16(2), ALU.mult)
        tt("dve", xsD, xsD.ap().rearrange("p (h q) -> p h q", h=16), xa, xs3v, vsm, dsk.unsqueeze(2).to_broadcast([128, 16, 64]), ALU.mult)
        if ci < 15:
            tt("pool", xdtd, xdtd.ap().rearrange("p (h q) -> p h q", h=16), xdt, xdt.ap().rearrange("p (h q) -> p h q", h=16),
               s16, bc16(6), ALU.mult)
        yield

    def back_head(c):
        b, ci = c // 16, c % 16
        if c == 0:
            dma("sp", pj, pj.ap(), B_proj[c], proj_d[c * 128:(c + 1) * 128, :])
        dma("sp", xt, xt.ap(), B_in, x_d[c * 128:(c + 1) * 128, :])
        if c + 1 < n_ch:
            load_xbc(c + 1)
        for g in range(2):
            pb = ps[g]
            mm(pb, pb.ap(), cbf, idb, xsD, xsD[:, g * 512:(g + 1) * 512], st=True, sp=False)
            for hh in range(8):
                h = g * 8 + hh
                mm(pb, pb[:, hh * 64:(hh + 1) * 64], Mh, Mh[:, h, :], xdt, xdt[:, h * 64:(h + 1) * 64], st=False, sp=(hh == 7))
        if ci > 0:
            for g in range(2):
                pb = ps[2 + g]
                sl = slice(g * 512, (g + 1) * 512)
                mm(pb, pb.ap(), BCT, BCT[:, 2 + g, :], stb, stb[:, sl])
                tt("dve", t1, t1[:, sl].rearrange("p (h q) -> p h q", h=8), pb, pb.ap().rearrange("p (h q) -> p h q", h=8),
                   s16, s16[:, 5, g * 8:(g + 1) * 8].unsqueeze(2).to_broadcast([128, 8, 64]), ALU.mult)
                tt("dve", yf, yf[:, sl], ps[g], ps[g].ap(), t1, t1[:, sl], ALU.add)
        else:
            for g in range(2):
                cp("dve", yf, yf[:, g * 512:(g + 1) * 512], ps[g], ps[g].ap())
        if ci < 15:
            for g in range(2):
                pb = ps[4 + g]
                sl = slice(g * 512, (g + 1) * 512)
                mm(pb, pb.ap(), xa, xa[:, 1024 + g * 128:1024 + (g + 1) * 128], xdtd, xdtd[:, sl])
                if ci > 0:
                    tt("pool", stf, stf[:, sl].rearrange("p (h q) -> p h q", h=8), stf, stf[:, sl].rearrange("p (h q) -> p h q", h=8),
                       s16, s16[:, 7, g * 8:(g + 1) * 8].unsqueeze(2).to_broadcast([128, 8, 64]), ALU.mult)
                    tt("dve", stf, stf[:, sl], pb, pb.ap(), stf, stf[:, sl], ALU.add)
                else:
                    cp("dve", stf, stf[:, sl], pb, pb.ap())
                cp("act", stb, stb[:, sl], stf, stf[:, sl])
        act(t1, t1.ap(), pj, pj[:, O_Z:O_Z + 1024], AF.Silu)
        tt("dve", yf, yf.ap(), yf, yf.ap(), t1, t1.ap(), ALU.mult)
        rmsnorm(yf, yf.ap(), ssdn, ssdn.ap(), ybr, ybr.ap(), "ssd", pool_pow=True)
        branch_proj(0, True)
        xattn(c)
        act(gu, gu.ap(), pj, pj[:, O_U:O_U + 1024], AF.Gelu_apprx_tanh)
        act(yf, yf.ap(), pj, pj[:, O_V:O_V + 1024], AF.Gelu_apprx_tanh)
        rmsnorm(yf, yf.ap(), gmlpn, gmlpn.ap(), xdt, xdt.ap(), "gmlp", pool_pow=True)
        for g in range(8):
            pb = ps[g // 4]
            mm(pb, pb[:, (g % 4) * 128:(g % 4 + 1) * 128], wsp, wsp[:, g, :], xdt, xdt[:, g * 128:(g + 1) * 128])
        for h2 in range(2):
            pb = ps[h2]
            sl = slice(h2 * 512, (h2 + 1) * 512)
            tt("dve", t1, t1[:, sl].rearrange("p (g q) -> p g q", g=4), pb, pb.ap().rearrange("p (g q) -> p g q", g=4),
               bsp, bsp[:, h2 * 4:(h2 + 1) * 4].unsqueeze(2).to_broadcast([128, 4, 128]), ALU.add)
            tt("pool", ybr, ybr[:, sl], t1, t1[:, sl], gu, gu[:, sl], ALU.mult)
        branch_proj(1, False)
        if c + 1 < n_ch:
            dma("sp", pj, pj.ap(), B_proj[c + 1], proj_d[(c + 1) * 128:(c + 2) * 128, :])

    def xattn(c):
        b = c // 16
        act(sg2, sg2.ap(), pj, pj[:, O_G + 2048:O_G + 3072], AF.Tanh, scale=0.5)
        transpose8(pj, lambda kc: pj[:, O_Q + kc * 128:O_Q + (kc + 1) * 128], bT2)
        for hd in range(4):
            pb = ps[5 + hd // 2]
            for j in range(2):
                mm(pb, pb[:, (hd % 2) * 256:(hd % 2 + 1) * 256], bT2, bT2[:, 2 * hd + j, :], KT, KT[:, b, 2 * hd + j, :], st=(j == 0), sp=(j == 1))
        for h2 in range(2):
            k.op("dve", lambda e, h2=h2: e.tensor_reduce(out=sm8[:, 2 * h2:2 * h2 + 2], in_=ps[5 + h2].ap().rearrange("p (a b) -> p a b", a=2),
                                                          axis=AX.X, op=ALU.max), reads=[ps[5 + h2]], writes=[sm8])
        ts("dve", sm8, sm8[:, 4:8], sm8, sm8[:, 0:4], -1.0 / 16.0, None, ALU.mult)
        for hd in range(4):
            pb = ps[5 + hd // 2]
            act(pexp, pexp3[:, hd, :], pb, pb[:, (hd % 2) * 256:(hd % 2 + 1) * 256], AF.Exp, bias=sm8[:, 4 + hd:5 + hd],
                scale=1.0 / 16.0, accum=sm8[:, 8 + hd:9 + hd], extra=[sm8], wextra=[sm8])
        k.op("dve", lambda e: e.reciprocal(out=sm8[:, 12:16], in_=sm8[:, 8:12]), reads=[sm8], writes=[sm8])
        transpose8(pexp, lambda kc: pexp3[:, kc // 2, (kc % 2) * 128:(kc % 2 + 1) * 128], bT2)
        for hd in range(4):
            pb = ps[5 + hd // 2]
            for j in range(2):
                mm(pb, pb[:, (hd % 2) * 256:(hd % 2 + 1) * 256], bT2, bT2[:, 2 * hd + j, :], Vm, Vm[:, b, j, hd * 256:(hd + 1) * 256], st=(j == 0), sp=(j == 1))
        for h2 in range(2):
            pb = ps[5 + h2]
            tt("dve", ybr2, ybr2[:, h2 * 512:(h2 + 1) * 512].rearrange("p (a q) -> p a q", a=2), pb, pb.ap().rearrange("p (a q) -> p a q", a=2),
               sm8, sm8[:, 12 + 2 * h2:14 + 2 * h2].unsqueeze(2).to_broadcast([128, 2, 256]), ALU.mult)

    def tail(c):
        b, ci = c // 16, c % 16
        branch_proj(2, False, sg=sg2, pre=True, src=ybr2)
        yield
        act(ybr, ybr.ap(), mg, mg.ap(), AF.Copy, scale=0.5)
        transpose8(ybr, lambda kc: ybr[:, kc * 128:(kc + 1) * 128], bT)
        yield
        for h2 in range(2):
            pb = ps[5 + h2]
            sl = slice(h2 * 512, (h2 + 1) * 512)
            for kc in range(8):
                mm(pb, pb.ap(), bT, bT[:, kc, :], wout, wout[:, kc, sl], st=(kc == 0), sp=(kc == 7))
            tt("dve", xt, xt[:, sl], pb, pb.ap(), xt, xt[:, sl], ALU.add)
        dma("sp", B_x1[c], x1_d[c * 128:(c + 1) * 128, :], xt, xt.ap())
        yield

    def interleave(*gens):
        gens = [g for g in gens if g is not None]
        while gens:
            for g in list(gens):
                try:
                    next(g)
                except StopIteration:
                    gens.remove(g)

    load_xbc(0)
    interleave(ssd_front(0))
    for c in range(n_ch):
        back_head(c)
        interleave(ssd_front(c + 1) if c + 1 < n_ch else None, tail(c))
    k.barrier()
    k.release()
    k.sb_hi = SB_HI
    if stop_after == 2:
        k.emit()
        return nc

    ITt = k.sb("ITt", [128, TOK], BF16)
    JTt = k.sb("JTt", [128, TOK], BF16)
    GTt = k.sb("GTt", [128, TOK], BF16)
    k.mark()
    wpq = k.sb("wpq", [128, 8, 2048], BF16)
    keysb = k.sb("keysb", [128, 16, 128], BF16)
    lnffn = k.sb("lnffn", [128, 1024], F32)
    xts = [k.sb("xt3_%d" % i, [128, 1024], F32) for i in range(2)]
    xnb = k.sb("xnb", [128, 1024], BF16)
    xnTc = [k.sb("xnTc%d" % i, [128, 8, 128], BF16) for i in range(2)]
    qpT = k.sb("qpT", [128, 16, 128], BF16)
    sc = k.sb("sc", [128, 16, 128], F32)
    scw = k.sb("scw", [128, 16, 128], F32)
    tv = k.sb("tv", [128, 16, 16], F32)
    ti = k.sb("ti", [128, 16, 16], U32)
    tif = k.sb("tif", [128, 16, 16], F32)
    cand = k.sb("cand", [128, 8, 256], F32)
    candw = k.sb("candw", [128, 8, 256], F32)
    bv = k.sb("bv", [128, 8, 16], F32)
    bpi = k.sb("bpi", [128, 8, 16], U32)
    bpa = k.sb("bpa", [128, 8, 16], U32)
    bpb = k.sb("bpb", [128, 8, 16], U32)
    bpf = k.sb("bpf", [128, 2, 128], F32)
    eq = cand
    eq4 = cand.ap().rearrange("p h (a b) -> p h a b", a=16)
    IJ = k.sb("IJ", [128, 3, 128], F32)
    IJb = k.sb("IJb", [128, 3, 128], BF16)
    gs = k.sb("gs", [128, 16], F32)
    dma("sp", wpq, wpq.ap(), B_wpqb, wpqb_d.ap().rearrange("(kc p) n -> p kc n", p=128))
    dma("pool", keysb, keysb.ap().rearrange("p a b -> p (a b)"), B_in, keys_d.ap(), max_dma_last_dim=4096)
    dma("sp", lnffn, lnffn.ap(), B_in, vecs_d[:, V_LNFFN:V_LNFFN + 1024])
    iota16 = cf[:, 384:400]
    tv_s = [tv.sub("g%d" % i) for i in range(16)]
    ti_s = [ti.sub("g%d" % i) for i in range(16)]
    scw_s = [scw.sub("g%d" % i) for i in range(16)]
    bv_s = [bv.sub("h%d" % i) for i in range(8)]
    bpi_s = [bpi.sub("h%d" % i) for i in range(8)]
    candw_s = [candw.sub("h%d" % i) for i in range(8)]
    scs = [sc, k.sb("sc_b", [128, 16, 128], F32)]
    xs3 = k.sb("xs3", [128, 1024], F32)
    sm3 = k.sb("sm3", [128, 8], F32)
    k.op("pool", lambda e: e.memset(sm3[:, 4:5], -0.5), writes=[sm3])

    def front(c):
        xt = xts[c % 2]
        xc = xnTc[c % 2]
        sc_ = scs[c % 2]
        dma("sp", xt, xt.ap(), B_x1[c], x1_d[c * 128:(c + 1) * 128, :])
        act(junk, junk.ap(), xt, xt.ap(), AF.Square, accum=sm3[:, 0:1], wextra=[sm3])
        ts("pool", sm3, sm3[:, 1:2], sm3, sm3[:, 0:1], 1.0 / D, EPS, ALU.mult, ALU.add)
        k.op("pool", lambda e: e.tensor_tensor(out=sm3[:, 2:3], in0=sm3[:, 1:2], in1=sm3[:, 4:5], op=ALU.pow), reads=[sm3], writes=[sm3])
        act(xs3, xs3.ap(), xt, xt.ap(), AF.Copy, scale=sm3[:, 2:3], extra=[sm3])
        tt("pool", xnb, xnb.ap(), xs3, xs3.ap(), lnffn, lnffn.ap(), ALU.mult)
        for kc in range(8):
            trn(ps[0], psb[0][:, kc * 128:(kc + 1) * 128], xnb, xnb[:, kc * 128:(kc + 1) * 128], idb)
        cp("act", xc, xc.ap().rearrange("p a b -> p (a b)"), ps[0], psb[0][:, :])
        dma("sp", B_xng[c], xng_d[c // 2, :, :, (c % 2) * 128:(c % 2 + 1) * 128], xc, xc.ap())
        for cc in range(16):
            pb = ps[1 + cc // 4]
            for kc in range(8):
                mm(pb, pb[:, (cc % 4) * 128:(cc % 4 + 1) * 128], wpq, wpq[:, kc, cc * 128:(cc + 1) * 128],
                   xc, xc[:, kc, :], st=(kc == 0), sp=(kc == 7))
        for q in range(4):
            cp("act", qpT, qpT[:, 4 * q:4 * q + 4, :].rearrange("p a b -> p (a b)"), ps[1 + q], ps[1 + q].ap())
        for cc in range(16):
            pb = ps[(5 + cc // 4) % 8] if cc < 12 else ps[1]
            mm(pb, pb[:, (cc % 4) * 128:(cc % 4 + 1) * 128], qpT, qpT[:, cc, :], keysb, keysb[:, cc, :])
        for q in range(4):
            pb = ps[(5 + q) % 8] if q < 3 else ps[1]
            cp("act", sc_, sc_[:, 4 * q:4 * q + 4, :].rearrange("p a b -> p (a b)"), pb, pb.ap())

    front(0)
    for c in range(n_ch):
        if c + 1 < n_ch:
            front(c + 1)
        sc = scs[c % 2]
        for cc in range(16):
            k.op("dve", lambda e, cc=cc, sc=sc: e.max(out=tv[:, cc, 0:8], in_=sc[:, cc, :]), reads=[sc], writes=[tv_s[cc]])
        for cc in range(16):
            k.op("dve", lambda e, cc=cc, sc=sc: e.max_index(out=ti[:, cc, 0:8], in_max=tv[:, cc, 0:8], in_values=sc[:, cc, :]), reads=[sc, tv_s[cc]], writes=[ti_s[cc]])
        for cc in range(16):
            k.op("dve", lambda e, cc=cc, sc=sc: e.match_replace(out=scw[:, cc, :], in_to_replace=tv[:, cc, 0:8], in_values=sc[:, cc, :], imm_value=-1e30),
                 reads=[sc, tv_s[cc]], writes=[scw_s[cc]])
        for cc in range(16):
            k.op("dve", lambda e, cc=cc, sc=sc: e.max(out=tv[:, cc, 8:16], in_=scw[:, cc, :]), reads=[scw_s[cc]], writes=[tv_s[cc]])
        for cc in range(16):
            k.op("dve", lambda e, cc=cc, sc=sc: e.max_index(out=ti[:, cc, 8:16], in_max=tv[:, cc, 8:16], in_values=scw[:, cc, :]), reads=[scw_s[cc], tv_s[cc]], writes=[ti_s[cc]])
        k.op("dve", lambda e: e.tensor_copy(out=tif.ap(), in_=ti.ap()), reads=ti_s, writes=[tif])
        tv4 = tv.ap().rearrange("p (h i) a -> p h i a", i=2)
        k.op("dve", lambda e, tv4=tv4: e.tensor_tensor(out=cand.ap().rearrange("p h (a b) -> p h a b", a=16),
                                                    in0=tv4[:, :, 0, :].unsqueeze(3).to_broadcast([128, 8, 16, 16]),
                                                    in1=tv4[:, :, 1, :].unsqueeze(2).to_broadcast([128, 8, 16, 16]), op=ALU.add),
             reads=tv_s, writes=[cand])
        for h in range(8):
            k.op("dve", lambda e, h=h: e.max(out=bv[:, h, 0:8], in_=cand[:, h, :]), reads=[cand], writes=[bv_s[h]])
        for h in range(8):
            k.op("dve", lambda e, h=h: e.max_index(out=bpi[:, h, 0:8], in_max=bv[:, h, 0:8], in_values=cand[:, h, :]), reads=[cand, bv_s[h]], writes=[bpi_s[h]])
        for h in range(8):
            k.op("dve", lambda e, h=h: e.match_replace(out=candw[:, h, :], in_to_replace=bv[:, h, 0:8], in_values=cand[:, h, :], imm_value=-1e30),
                 reads=[cand, bv_s[h]], writes=[candw_s[h]])
        for h in range(8):
            k.op("dve", lambda e, h=h: e.max(out=bv[:, h, 8:16], in_=candw[:, h, :]), reads=[candw_s[h]], writes=[bv_s[h]])
        for h in range(8):
            k.op("dve", lambda e, h=h: e.max_index(out=bpi[:, h, 8:16], in_max=bv[:, h, 8:16], in_values=candw[:, h, :]), reads=[candw_s[h], bv_s[h]], writes=[bpi_s[h]])
        k.op("dve", lambda e: e.tensor_scalar(out=bpa.ap(), in0=bpi.ap(), scalar1=4, scalar2=None, op0=ALU.logical_shift_right), reads=bpi_s, writes=[bpa])
        k.op("dve", lambda e: e.tensor_scalar(out=bpb.ap(), in0=bpi.ap(), scalar1=15, scalar2=None, op0=ALU.bitwise_and), reads=bpi_s, writes=[bpb])
        cp("dve", bpf, bpf[:, 0, :], bpa, bpa.ap().rearrange("p a b -> p (a b)"))
        cp("dve", bpf, bpf[:, 1, :], bpb, bpb.ap().rearrange("p a b -> p (a b)"))
        tif4 = tif.ap().rearrange("p (h i) a -> p h i a", i=2)
        for w_ in range(2):
            pos = bpf[:, w_, :].rearrange("p (h k) -> p h k", h=8)
            tt("dve", eq, eq4, bpf, pos.unsqueeze(3).to_broadcast([128, 8, 16, 16]),
               cf, iota16.unsqueeze(1).unsqueeze(1).to_broadcast([128, 8, 16, 16]), ALU.is_equal)
            tt("dve", eq, eq4, eq, eq4, tif, tif4[:, :, w_, :].unsqueeze(2).to_broadcast([128, 8, 16, 16]), ALU.mult)
            k.op("dve", lambda e, w_=w_: e.tensor_reduce(out=IJ[:, w_, :], in_=cand.ap().rearrange("p h (k a) -> p (h k) a", a=16), axis=AX.X, op=ALU.add),
                 reads=[eq], writes=[IJ])
        g3 = IJ[:, 2, :].rearrange("p (h k) -> p h k", h=8)
        k.op("dve", lambda e, g3=g3: e.tensor_tensor(out=g3, in0=bv.ap(), in1=bv[:, :, 0:1].to_broadcast([128, 8, 16]), op=ALU.subtract),
             reads=bv_s, writes=[IJ])
        act(IJ, IJ[:, 2, :], IJ, IJ[:, 2, :], AF.Exp)
        k.op("dve", lambda e: e.tensor_reduce(out=gs[:, 0:8], in_=g3, axis=AX.X, op=ALU.add), reads=[IJ], writes=[gs])
        k.op("dve", lambda e: e.reciprocal(out=gs[:, 8:16], in_=gs[:, 0:8]), reads=[gs], writes=[gs])
        tt("dve", IJ, g3, IJ, g3, gs, gs[:, 8:16].unsqueeze(2).to_broadcast([128, 8, 16]), ALU.mult)
        cp("dve", IJb, IJb.ap(), IJ, IJ.ap())
        for w_ in range(3):
            trn(ps[0], psb[0][:, w_ * 128:(w_ + 1) * 128], IJb, IJb[:, w_, :], idb)
        for w_, dst in enumerate((ITt, JTt, GTt)):
            cp("act", dst, dst[:, c * 128:(c + 1) * 128], ps[0], psb[0][:, w_ * 128:(w_ + 1) * 128])
    k.barrier()
    k.release()
    if stop_after == 3:
        dump("ITt", ITt, ITt[:, 0:ntok], [128, ntok])
        dump("JTt", JTt, JTt[:, 0:ntok], [128, ntok])
        dump("GTt", GTt, GTt[:, 0:ntok], [128, ntok])
        k.emit()
        return nc

    k.mark()
    TG = 256
    Gs = [k.sb("Gs%d" % i, [128, TG, 128], BF16) for i in range(2)]
    xg = [k.sb("xg%d" % i, [128, 8, TG], BF16) for i in range(2)]
    lnfin = k.sb("lnfin", [128, 1024], F32)
    iob = cbf[:, C_IOTA:C_IOTA + 128]
    NB4 = 5
    UT = [k.sb("UT%d" % i, [128, 8, 128], BF16) for i in range(NB4)]
    Vi = [k.sb("Vi%d" % i, [128, 1024], BF16) for i in range(NB4)]
    Rt = [k.sb("Rt%d" % i, [128, 128], BF16) for i in range(4)]
    Ct = [k.sb("Ct%d" % i, [128, 128], BF16) for i in range(4)]
    ga = [k.sb("ga%d" % i, [128, TG], BF16) for i in range(2)]
    Hh = [k.sb("Hh%d" % i, [128, TG], BF16) for i in range(3)]
    xt4 = [k.sb("xt4_%d" % i, [128, 1024], F32) for i in range(2)]
    yo = [k.sb("yo%d" % i, [128, 1024], F32) for i in range(1)]
    dma("pool", lnfin, lnfin.ap(), B_in, vecs_d[:, V_LNFIN:V_LNFIN + 1024])
    ngrp = ntok // TG

    def load_xg(gq):
        dst = xg[gq % 2]
        k.op("pool", lambda e: e.dma_start(out=dst.ap(), in_=xng_d[gq]), reads=[B_xng[2 * gq], B_xng[2 * gq + 1]], writes=[dst], dma=True)

    def g_tokens(gq, tl, defer=None):
        G_ = Gs[gq % 2]
        for tloc in tl:
            s_, tq = tloc % 4, tloc // 4
            pb = ps[7]
            t = gq * TG + tloc
            r_, c_ = Rt[s_], Ct[s_]
            ts("dve", r_, r_.ap(), cbf, iob, ITt[:, t:t + 1], GTt[:, t:t + 1], ALU.is_equal, ALU.mult, extra=[ITt, GTt])
            ts("dve", c_, c_.ap(), cbf, iob, JTt[:, t:t + 1], None, ALU.is_equal, extra=[JTt])
            mm(pb, pb[:, s_ * 128:(s_ + 1) * 128], c_, c_.ap(), r_, r_.ap())
            if s_ == 3:
                if defer is None:
                    cp("act", G_, G_[:, tq * 4:tq * 4 + 4, :].rearrange("p t i -> p (t i)"), pb, pb.ap())
                else:
                    defer.append((G_, G_[:, tq * 4:tq * 4 + 4, :].rearrange("p t i -> p (t i)"), pb))

    def issue_loads(gq, i):
        u_, v_ = UT[i % NB4], Vi[i % NB4]
        dma("sp", u_, u_.ap().rearrange("p a b -> p (a b)"), B_ub[i // 8], ub_d[i * 128:(i + 1) * 128, :])
        dma("sp", v_, v_.ap(), B_vb[i // 8], vb_d[i * 128:(i + 1) * 128, :])

    def a_mm(gq, i):
        u_ = UT[i % NB4]
        pa = ps[4 + i % 3]
        x_ = xg[gq % 2]
        for kc in range(8):
            mm(pa, pa[:, 0:TG], u_, u_[:, kc, :], x_, x_[:, kc, :], st=(kc == 0), sp=(kc == 7))

    def gelu_mult(gq, i):
        pa = ps[4 + i % 3]
        g_, h_ = ga[i % 2], Hh[i % 3]
        act(g_, g_.ap(), pa, pa[:, 0:TG], AF.Gelu_apprx_tanh)
        tt("dve", h_, h_.ap(), g_, g_.ap(), Gs[gq % 2], Gs[gq % 2][:, :, i], ALU.mult)

    load_xg(0)
    g_tokens(0, range(TG))
    pending_fin = []

    def make_fin(t0):
        def fin():
            for tb in range(TG // 128):
                cidx = (t0 // 128) + tb
                xt, y_ = xt4[tb % 2], yo[0]
                rmsnorm(xt, xt.ap(), lnfin, lnfin.ap(), y_, y_.ap(), "fin", pool_pow=True)
                dma("pool", B_out, out_d[cidx * 128:(cidx + 1) * 128, :], y_, y_.ap())
        return fin

    for gq in range(ngrp):
        t0 = gq * TG
        if gq + 1 < ngrp:
            load_xg(gq + 1)
        issue_loads(gq, 0)
        a_mm(gq, 0)
        issue_loads(gq, 1)
        a_mm(gq, 1)
        gelu_mult(gq, 0)
        for i in range(128):
            if i + 2 < 128:
                issue_loads(gq, i + 2)
                a_mm(gq, i + 2)
            dfr = []
            if gq + 1 < ngrp:
                g_tokens(gq + 1, [2 * i, 2 * i + 1], dfr)
            if i + 1 < 128:
                gelu_mult(gq, i + 1)
            for (gb_, gap_, pb_) in dfr:
                cp("act", gb_, gap_, pb_, pb_.ap())
            v_ = Vi[i % NB4]
            h_ = Hh[i % 3]
            for tb in range(TG // 128):
                for dh in range(2):
                    pb = ps[tb * 2 + dh]
                    mm(pb, pb.ap(), h_, h_[:, tb * 128:(tb + 1) * 128], v_, v_[:, dh * 512:(dh + 1) * 512], st=(i == 0), sp=(i == 127))
            if i == 3:
                while pending_fin:
                    pending_fin.pop(0)()
                for tb in range(TG // 128):
                    cidx = (t0 // 128) + tb
                    dma("pool", xt4[tb % 2], xt4[tb % 2].ap(), B_x1[cidx], x1_d[cidx * 128:(cidx + 1) * 128, :])
        for tb in range(TG // 128):
            xt = xt4[tb % 2]
            for dh in range(2):
                pb = ps[tb * 2 + dh]
                tt("dve", xt, xt[:, dh * 512:(dh + 1) * 512], pb, pb.ap(), xt, xt[:, dh * 512:(dh + 1) * 512], ALU.add)
        pending_fin.append(make_fin(t0))
    while pending_fin:
        pending_fin.pop(0)()
    k.op("pool", None, reads=[B_out])
    k.barrier()
    k.release()
    k.emit()
    return nc


def host_consts():
    c = np.zeros((128, NC_CONST), np.float32)
    r = np.arange(128)
    c[:, C_ID:C_ID + 128] = np.eye(128)
    c[:, C_TRIU:C_TRIU + 128] = (r[:, None] <= r[None, :])
    c[:, C_ONES:C_ONES + 128] = 1.0
    for kk in range(3):
        c[:, C_S + kk * 128:C_S + (kk + 1) * 128] = (r[None, :] == r[:, None] + (3 - kk))
        c[:, C_P + kk * 128:C_P + (kk + 1) * 128] = (r[None, :] == r[:, None] + (3 - kk) - 128)
    c[:, C_NEG:C_NEG + 128] = np.where(r[None, :] < r[:, None], -30000.0, 0.0)
    c[:, C_IOTA:C_IOTA + 128] = r[None, :]
    c[0, C_R0:C_R0 + 128] = 1.0
    return c


def prep_inputs(x, mem, ln_mix, w_in, conv_w, conv_b, dt_bias, a_log, d_skip, ssd_norm, gmlp_norm, w_spatial,
                b_spatial, ln_mem, w_mem_kv, w_branch, w_out, ln_ffn, w_peer_q, peer_keys, peer_u, peer_v, ln_final):
    f = lambda a: np.ascontiguousarray(np.asarray(a, dtype=np.float32))
    vec = np.concatenate([f(ln_mix[0]), f(conv_w[0]).reshape(-1), f(conv_b[0]), f(dt_bias[0]), f(a_log[0]), f(d_skip[0]),
                          f(ssd_norm[0]), f(gmlp_norm[0]), f(ln_mem[0]), f(ln_ffn[0]), f(ln_final)])
    assert vec.shape[0] == NV
    vecs = np.ascontiguousarray(np.broadcast_to(vec[None, :], (128, NV)))
    keys = f(peer_keys[0])
    keysT = np.ascontiguousarray(keys.transpose(3, 0, 1, 2).reshape(128, 2048))
    wsp = f(w_spatial[0])
    wspT = np.ascontiguousarray(wsp.transpose(2, 0, 1).reshape(128, 1024))
    U = f(peer_u[0])
    Uh = np.ascontiguousarray(U.reshape(128, 128, 8, 128).transpose(0, 3, 2, 1).reshape(16384, 1024))
    shared = {
        "w_in": f(w_in[0]), "w_kv": f(w_mem_kv[0]), "w_br": f(w_branch[0]).reshape(3 * D, D), "w_out": f(w_out[0]),
        "w_pq": f(w_peer_q[0]), "keysT": keysT, "wspT": wspT, "Uh": Uh, "V": f(peer_v[0]), "vecs": vecs,
        "consts": host_consts(), "bsp": np.ascontiguousarray(f(b_spatial[0]).T),
    }
    xs = f(x).reshape(NCORES, TOK, D)
    ms = f(mem).reshape(NCORES, 512, D)
    in_maps = []
    for i in range(NCORES):
        m = dict(shared)
        m["x"] = xs[i]
        m["mem"] = ms[i]
        in_maps.append(m)
    return in_maps


def kernel(**inputs):
    in_maps = prep_inputs(**inputs)
    nc = build_nc()
    res = run_bass_kernel_spmd(nc, in_maps, core_ids=list(range(NCORES)))
    out = np.stack([np.asarray(r["out"], dtype=np.float32) for r in res.results], axis=0)
    return out.reshape(16, 2048, D)
```

```python
import numpy as np
import concourse.bass as bass
import concourse.mybir as mybir
from concourse.bass_utils import run_bass_kernel_spmd

F32 = mybir.dt.float32
BF16 = mybir.dt.bfloat16
U32 = mybir.dt.uint32
ALU = mybir.AluOpType
AF = mybir.ActivationFunctionType
AX = mybir.AxisListType

SB_LO = 16512
SB_HI = 229344
NDMA_SEM = 32
ENGS = ("pe", "dve", "act", "pool", "sp")

NCORES = 8
TOK = 4096
NCH = 32
D = 1024
IN_DIM = 8720
EPS = 1e-6
O_Z, O_XBC, O_DT, O_U, O_V, O_Q, O_G = 0, 1024, 2560, 2576, 3600, 4624, 5648
C_ID, C_TRIU, C_ONES, C_S, C_P, C_NEG, C_IOTA, C_R0, NC_CONST = 0, 128, 256, 384, 768, 1152, 1280, 1408, 1536
V_LNMIX, V_CONVW, V_CONVB, V_DTB, V_ALOG, V_DSKIP, V_SSDN, V_GMLPN, V_LNMEM, V_LNFFN, V_LNFIN, NV = (
    0, 1024, 7168, 8704, 8720, 8736, 8752, 9776, 10800, 11824, 12848, 13872)


class Buf:
    __slots__ = ("name", "writers", "readers", "t")

    def __init__(self, name, t=None):
        self.name = name
        self.writers = {}
        self.readers = {}
        self.t = t

    def sub(self, name):
        return Buf(self.name + "." + name, self.t)

    def __getitem__(self, key):
        return self.t[key]

    def ap(self):
        return self.t.ap()


class Op:
    __slots__ = ("eng", "fn", "deps", "is_dma", "signal", "sem", "val", "gidx")

    def __init__(self, eng, fn, is_dma, gidx):
        self.eng = eng
        self.fn = fn
        self.is_dma = is_dma
        self.deps = []
        self.signal = False
        self.sem = None
        self.val = 0
        self.gidx = gidx


class K:
    def __init__(self, nc):
        self.nc = nc
        self.ops = {e: [] for e in ENGS}
        self.all_ops = []
        self.sb_ptr = SB_LO
        self.marks = []
        self.n_dma = 0
        self.n_dma_sw = 0
        self.dma_last = [None] * NDMA_SEM
        self.dma_cnt = [0] * NDMA_SEM
        self.dma_sems = [nc.alloc_semaphore("dsem%d" % i) for i in range(NDMA_SEM)]
        self.eng_sems = {e: nc.alloc_semaphore("esem_" + e) for e in ENGS}
        self.pending_dma = []
        self.nameid = 0
        self.sb_hi = SB_HI

    def sb_top(self, name, shape, dtype):
        esz = {F32: 4, BF16: 2, U32: 4}[dtype]
        n = 1
        for s in shape[1:]:
            n *= s
        nbytes = (n * esz + 31) // 32 * 32
        self.sb_hi -= nbytes
        assert self.sb_hi >= self.sb_ptr
        self.nameid += 1
        t = self.nc.alloc_sbuf_tensor_at("%s_%d" % (name, self.nameid), list(shape), dtype, offset=self.sb_hi)
        return Buf(name, t)

    def sb(self, name, shape, dtype):
        esz = {F32: 4, BF16: 2, U32: 4}[dtype]
        n = 1
        for s in shape[1:]:
            n *= s
        nbytes = (n * esz + 31) // 32 * 32
        off = self.sb_ptr
        assert off + nbytes <= self.sb_hi, "SBUF overflow at %s: need %d have %d" % (name, nbytes, self.sb_hi - off)
        self.sb_ptr += nbytes
        self.nameid += 1
        t = self.nc.alloc_sbuf_tensor_at("%s_%d" % (name, self.nameid), list(shape), dtype, offset=off)
        return Buf(name, t)

    def mark(self):
        self.marks.append(self.sb_ptr)

    def release(self):
        self.sb_ptr = self.marks.pop()

    def op(self, eng, fn, reads=(), writes=(), dma=False):
        o = Op(eng, fn, dma, len(self.all_ops))
        deps = {}
        for b in reads:
            for w in b.writers.values():
                deps[w] = True
        for b in writes:
            for w in b.writers.values():
                deps.setdefault(w, False)
            for r in b.readers.values():
                deps.setdefault(r, False)
        if dma:
            if eng == "pool":
                si = NDMA_SEM - 8 + self.n_dma_sw % 8
                self.n_dma_sw += 1
            else:
                si = self.n_dma % (NDMA_SEM - 8)
                self.n_dma += 1
            prev = self.dma_last[si]
            if prev is not None:
                deps.setdefault(prev, True)
            self.dma_last[si] = o
            self.dma_cnt[si] += 1
            o.sem = self.dma_sems[si]
            o.val = 16 * self.dma_cnt[si]
            o.signal = True
            self.pending_dma.append(o)
        for d, raw in deps.items():
            if d is o:
                continue
            if (not d.is_dma) and (not dma) and d.eng == eng:
                if eng == "pe":
                    continue
            o.deps.append(d)
        key = ("dma", o.gidx) if dma else eng
        for b in reads:
            b.readers[key] = o
        for b in writes:
            b.writers = {key: o}
            b.readers = {}
        self.ops[eng].append(o)
        self.all_ops.append(o)
        return o

    def barrier(self):
        lasts = []
        for e in ENGS:
            for o in reversed(self.ops[e]):
                if (not o.is_dma) and o.fn is not None:
                    lasts.append(o)
                    break
        pend = list(self.pending_dma)
        self.pending_dma = []
        for e in ENGS:
            o = Op(e, None, False, len(self.all_ops))
            for d in lasts + pend:
                if d.eng == e and not d.is_dma:
                    continue
                o.deps.append(d)
            self.ops[e].append(o)
            self.all_ops.append(o)

    def emit(self):
        nc = self.nc
        for o in self.all_ops:
            for d in o.deps:
                d.signal = True
        for e in ENGS:
            c = 0
            for o in self.ops[e]:
                if o.is_dma:
                    continue
                if o.signal:
                    c += 1
                    o.sem = self.eng_sems[e]
                    o.val = c

        def run(engname):
            def body(e):
                waited = {}
                for o in self.ops[engname]:
                    for d in o.deps:
                        s, v = d.sem, d.val
                        if waited.get(s.num, 0) >= v:
                            continue
                        e.wait_ge(s, v)
                        waited[s.num] = v
                    if o.fn is None:
                        continue
                    ins = o.fn(e)
                    if o.signal:
                        ins.then_inc(o.sem, 16 if o.is_dma else 1)
            return body

        with nc.Block() as block:
            block.sync(run("sp"))
            block.scalar(run("act"))
            block.tensor(run("pe"))
            block.vector(run("dve"))
            block.gpsimd(run("pool"))


def build_nc(n_ch=NCH, stop_after=None, debug=False):
    nc = bass.Bass("TRN2", target_bir_lowering=False)
    k = K(nc)
    ntok = n_ch * 128

    def din(name, shape, dt=F32):
        return nc.dram_tensor(name, list(shape), dt, kind="ExternalInput")

    x_d = din("x", [TOK, D]); mem_d = din("mem", [512, D])
    win_d = din("w_in", [D, IN_DIM]); wkv_d = din("w_kv", [D, 2048]); wbr_d = din("w_br", [3 * D, D])
    wout_d = din("w_out", [D, D]); wpq_d = din("w_pq", [D, 2048]); keys_d = din("keysT", [128, 2048])
    wsp_d = din("wspT", [128, 1024]); uh_d = din("Uh", [16384, D]); v_d = din("V", [16384, D])
    vecs_d = din("vecs", [128, NV]); consts_d = din("consts", [128, NC_CONST]); bsp_d = din("bsp", [128, 8])
    out_d = nc.dram_tensor("out", [TOK, D], F32, kind="ExternalOutput")
    proj_d = nc.dram_tensor("proj_s", [TOK, IN_DIM], BF16, kind="Internal")
    dtraw_d = nc.dram_tensor("dtraw_s", [TOK, 16], F32, kind="Internal")
    x1_d = nc.dram_tensor("x1_s", [TOK, D], F32, kind="Internal")
    ub_d = nc.dram_tensor("ub_s", [16384, D], BF16, kind="Internal")
    vb_d = nc.dram_tensor("vb_s", [16384, D], BF16, kind="Internal")
    xng_d = nc.dram_tensor("xng_s", [NCH // 2, 128, 8, 256], BF16, kind="Internal")
    wbrb_d = nc.dram_tensor("wbrb_s", [3 * D, D], BF16, kind="Internal")
    woutb_d = nc.dram_tensor("woutb_s", [D, D], BF16, kind="Internal")
    wpqb_d = nc.dram_tensor("wpqb_s", [D, 2048], BF16, kind="Internal")
    dbg = {}
    if debug:
        for nm, shp in debug.items():
            dbg[nm] = nc.dram_tensor("dbg_" + nm, list(shp), F32, kind="ExternalOutput")

    B_proj = [Buf("proj%d" % c) for c in range(NCH)]
    B_dtraw = [Buf("dtraw%d" % c) for c in range(NCH)]
    B_x1 = [Buf("x1_%d" % c) for c in range(NCH)]
    B_ub = [Buf("ub%d" % i) for i in range(16)]
    B_vb = [Buf("vb%d" % i) for i in range(16)]
    B_out = Buf("out")
    B_wbrb = [Buf("wbrb%d" % i) for i in range(3)]
    B_woutb = Buf("woutb")
    B_wpqb = Buf("wpqb")
    B_xng = [Buf("xng%d" % c) for c in range(NCH)]
    B_in = Buf("inputs")

    ps = [Buf("ps%d" % i, nc.alloc_psum_tensor("ps%d" % i, [128, 512], F32)) for i in range(8)]
    psb = [p.t.bitcast(BF16) for p in ps]

    def mm(pb, pout, lb, lap, rb, rap, st=True, sp=True):
        k.op("pe", lambda e: e.matmul(pout, lap, rap, start=st, stop=sp), reads=[lb, rb], writes=[pb])

    def trn(pb, pout, ib, iap, idap):
        k.op("pe", lambda e: e.transpose(pout, iap, idap), reads=[ib, cbf], writes=[pb])

    def act(ob, oap, ib, iap, func, bias=None, scale=1.0, accum=None, extra=(), wextra=()):
        kw = {}
        if bias is not None:
            kw["bias"] = bias
        if accum is not None:
            kw["accum_out"] = accum
        k.op("act", lambda e: e.activation(out=oap, in_=iap, func=func, scale=scale, **kw),
             reads=[ib] + list(extra), writes=[ob] + list(wextra))

    def tt(eng, ob, oap, ab, aap, bb, bap, op):
        k.op(eng, lambda e: e.tensor_tensor(out=oap, in0=aap, in1=bap, op=op), reads=[ab, bb], writes=[ob])

    def ts(eng, ob, oap, ab, aap, s1, s2, op0, op1=ALU.bypass, extra=()):
        if s2 is None:
            k.op(eng, lambda e: e.tensor_scalar(out=oap, in0=aap, scalar1=s1, scalar2=None, op0=op0),
                 reads=[ab] + list(extra), writes=[ob])
        else:
            k.op(eng, lambda e: e.tensor_scalar(out=oap, in0=aap, scalar1=s1, scalar2=s2, op0=op0, op1=op1),
                 reads=[ab] + list(extra), writes=[ob])

    def stt(ob, oap, ab, aap, scalar, bb, bap, op0, op1, extra=()):
        k.op("dve", lambda e: e.scalar_tensor_tensor(out=oap, in0=aap, scalar=scalar, in1=bap, op0=op0, op1=op1),
             reads=[ab, bb] + list(extra), writes=[ob])

    def cp(eng, ob, oap, ib, iap):
        if eng == "act":
            k.op("act", lambda e: e.activation(out=oap, in_=iap, func=AF.Copy), reads=[ib], writes=[ob])
        else:
            k.op(eng, lambda e: e.tensor_copy(out=oap, in_=iap), reads=[ib], writes=[ob])

    def dma(q, ob, oap, ib, iap, nobar=False, **kw):
        o = k.op(q, lambda e: e.dma_start(out=oap, in_=iap, **kw), reads=[ib], writes=[ob], dma=True)
        if nobar:
            k.pending_dma.remove(o)

    def dump(name, b, ap_, shape):
        if name in dbg:
            k.mark()
            tmp = k.sb("dbgtmp", list(shape), F32)
            cp("dve", tmp, tmp.ap(), b, ap_)
            dma("sp", B_out, dbg[name].ap(), tmp, tmp.ap())
            k.op("sp", None, reads=[B_out])
            k.barrier()
            k.release()

    cf = k.sb("cf", [128, 384 + 16], F32)
    cbf = k.sb("cbf", [128, NC_CONST], BF16)
    dma("sp", cf, cf[:, 0:384], B_in, consts_d[:, 0:384])
    dma("sp", cf, cf[:, 384:400], B_in, consts_d[:, C_IOTA:C_IOTA + 16])
    dma("pool", cbf, cbf.ap(), B_in, consts_d.ap(), max_dma_last_dim=4096)
    idb = cbf[:, C_ID:C_ID + 128]
    idf = cf[:, C_ID:C_ID + 128]
    small = k.sb("small", [128, 64], F32)
    junk = k.sb("junk", [128, 1024], BF16)

    k.op("dve", lambda e: e.memset(small[:, 8:9], -0.5), writes=[small])

    def rmsnorm(xb, xap, gb, gap, ob, oap, tag, pool_pow=False):
        act(junk, junk.ap(), xb, xap, AF.Square, accum=small[:, 0:1], wextra=[small])
        ts("dve", small, small[:, 1:2], small, small[:, 0:1], 1.0 / D, EPS, ALU.mult, ALU.add)
        if pool_pow:
            k.op("pool", lambda e: e.tensor_tensor(out=small[:, 3:4], in0=small[:, 1:2], in1=small[:, 8:9], op=ALU.pow),
                 reads=[small], writes=[small])
        else:
            act(small, small[:, 2:3], small, small[:, 1:2], AF.Sqrt)
            k.op("dve", lambda e: e.reciprocal(out=small[:, 3:4], in_=small[:, 2:3]), reads=[small], writes=[small])
        stt(ob, oap, xb, xap, small[:, 3:4], gb, gap, ALU.mult, ALU.mult, extra=[small])

    KT = k.sb_top("KT", [128, 2, 8, 256], BF16)
    Vm = k.sb_top("Vm", [128, 2, 2, 1024], BF16)


    k.mark()
    win = k.sb("win", [128, 8, IN_DIM], BF16)
    win_nb = [win.sub("c%d" % nb) for nb in range(18)]
    k.mark()
    wkv = k.sb("wkv", [128, 8, 2048], BF16)
    lnmem = k.sb("lnmem", [128, 1024], F32)
    memt = k.sb("memt", [128, 1024], F32)
    memn = k.sb("memn", [128, 1024], BF16)
    memT = k.sb("memT", [128, 8, 128], BF16)
    for kc in range(8):
        dma("pool", wkv, wkv[:, kc, :], B_in, wkv_d[kc * 128:(kc + 1) * 128, :], max_dma_last_dim=4096)
    for nb in range(18):
        c0_ = nb * 512
        w_ = min(512, IN_DIM - c0_)
        dma("pool", win_nb[nb], win[:, :, c0_:c0_ + w_], B_in, win_d[:, c0_:c0_ + w_].rearrange("(kc p) n -> p kc n", p=128),
            nobar=True, max_dma_last_dim=4096)
    dma("sp", lnmem, lnmem.ap(), B_in, vecs_d[:, V_LNMEM:V_LNMEM + 1024])
    for mt in range(4):
        b, j = mt // 2, mt % 2
        dma("sp", memt, memt.ap(), B_in, mem_d[mt * 128:(mt + 1) * 128, :])
        rmsnorm(memt, memt.ap(), lnmem, lnmem.ap(), memn, memn.ap(), "mem")
        for kc in range(8):
            trn(ps[0], psb[0][:, kc * 128:(kc + 1) * 128], memn, memn[:, kc * 128:(kc + 1) * 128], idb)
        cp("dve", memT, memT.ap().rearrange("p a b -> p (a b)"), ps[0], psb[0][:, :])
        for dc in range(8):
            pb = ps[1 + dc // 4]
            for kc in range(8):
                mm(pb, pb[:, (dc % 4) * 128:(dc % 4 + 1) * 128], wkv, wkv[:, kc, dc * 128:(dc + 1) * 128],
                   memT, memT[:, kc, :], st=(kc == 0), sp=(kc == 7))
        for h2 in range(2):
            pb = ps[1 + h2]
            cp("act", KT, KT[:, b, h2 * 4:(h2 + 1) * 4, j * 128:(j + 1) * 128],
               pb, pb.ap().rearrange("p (a b) -> p a b", a=4))
        for h2 in range(2):
            pb = ps[3 + h2]
            for kc in range(8):
                mm(pb, pb.ap(), memT, memT[:, kc, :], wkv, wkv[:, kc, 1024 + h2 * 512:1024 + (h2 + 1) * 512],
                   st=(kc == 0), sp=(kc == 7))
            cp("dve", Vm, Vm[:, b, j, h2 * 512:(h2 + 1) * 512], pb, pb.ap())
    k.barrier()
    k.release()
    if stop_after == 0:
        dump("KT", KT, KT.ap().rearrange("p a b c -> p (a b c)"), [128, 4096])
        dump("Vm", Vm, Vm.ap().rearrange("p a b c -> p (a b c)"), [128, 4096])
        k.emit()
        return nc

    k.mark()
    lnmix = k.sb("lnmix", [128, 1024], F32)
    xts = [k.sb("xt%d" % i, [128, 1024], F32) for i in range(1)]
    hb = k.sb("hb", [128, 1024], BF16)
    hTs = [k.sb("hT%d" % i, [128, 8, 128], BF16) for i in range(1)]
    stg = [k.sb("stg%d" % i, [128, IN_DIM], BF16) for i in range(2)]
    stg_sub = [[stg[i].sub("b%d" % nb) for nb in range(18)] for i in range(2)]
    dts = [k.sb("dts%d" % i, [128, 16], F32) for i in range(2)]
    dma("sp", lnmix, lnmix.ap(), B_in, vecs_d[:, V_LNMIX:V_LNMIX + 1024])
    for i in range(3):
        dma("pool", B_wbrb[i], wbrb_d[i * 1024:(i + 1) * 1024, :], B_in, wbr_d[i * 1024:(i + 1) * 1024, :], max_dma_last_dim=4096)
    dma("pool", B_woutb, woutb_d.ap(), B_in, wout_d.ap(), max_dma_last_dim=4096)
    dma("pool", B_wpqb, wpqb_d.ap(), B_in, wpq_d.ap(), max_dma_last_dim=4096)
    for i in range(16):
        dma("pool", B_ub[i], ub_d[i * 1024:(i + 1) * 1024, :], B_in, uh_d[i * 1024:(i + 1) * 1024, :], max_dma_last_dim=4096)
        dma("pool", B_vb[i], vb_d[i * 1024:(i + 1) * 1024, :], B_in, v_d[i * 1024:(i + 1) * 1024, :], max_dma_last_dim=4096)
    NBLK = 18
    for c in range(n_ch):
        par = c % 2
        xt, hT, st_, dt_ = xts[0], hTs[0], stg[par], dts[par]
        if c == 0:
            dma("sp", xt, xt.ap(), B_in, x_d[0:128, :])
        rmsnorm(xt, xt.ap(), lnmix, lnmix.ap(), hb, hb.ap(), "mix")
        if c + 1 < n_ch:
            dma("sp", xt, xt.ap(), B_in, x_d[(c + 1) * 128:(c + 2) * 128, :])
        for kc in range(8):
            trn(ps[0], psb[0][:, kc * 128:(kc + 1) * 128], hb, hb[:, kc * 128:(kc + 1) * 128], idb)
        cp("dve", hT, hT.ap().rearrange("p a b -> p (a b)"), ps[0], psb[0][:, :])
        subs = []
        for nb in range(NBLK):
            c0 = nb * 512
            w = min(512, IN_DIM - c0)
            pb = ps[1 + nb % 7]
            for kc in range(8):
                mm(pb, pb[:, 0:w], hT, hT[:, kc, :], win_nb[nb], win[:, kc, c0:c0 + w], st=(kc == 0), sp=(kc == 7))
            sbuf_ = stg_sub[par][nb]
            subs.append(sbuf_)
            cp("act" if nb % 2 == 0 else "dve", sbuf_, st_[:, c0:c0 + w], pb, pb[:, 0:w])
            if nb == 5:
                cp("dve", dt_, dt_.ap(), pb, pb[:, 0:16])
        k.op("sp", lambda e, c=c, st_=st_: e.dma_start(out=proj_d[c * 128:(c + 1) * 128, :], in_=st_.ap()),
             reads=subs, writes=[B_proj[c]], dma=True)
        dma("sp", B_dtraw[c], dtraw_d[c * 128:(c + 1) * 128, :], dt_, dt_.ap())
    k.barrier()
    k.release()
    k.release()
    if stop_after == 1:
        k.emit()
        return nc

    k.mark()
    wbr = k.sb("wbr", [128, 24, 1024], BF16)
    wout = k.sb("wout", [128, 8, 1024], BF16)
    wsp = k.sb("wsp", [128, 8, 128], BF16)
    convw = k.sb("convw", [128, 4, 1536], BF16)
    convb = k.sb("convb", [128, 1536], BF16)
    vsm = k.sb("vsm", [128, 48], F32)
    aneg = k.sb("aneg", [128, 16], F32)
    ssdn = k.sb("ssdn", [128, 1024], F32)
    gmlpn = k.sb("gmlpn", [128, 1024], F32)
    bsp = k.sb("bsp", [128, 8], F32)
    for i in range(3):
        dma("sp", wbr, wbr[:, i * 8:(i + 1) * 8, :], B_wbrb[i], wbrb_d[i * 1024:(i + 1) * 1024, :].rearrange("(kc p) n -> p kc n", p=128))
    dma("sp", wout, wout.ap(), B_woutb, woutb_d.ap().rearrange("(kc p) n -> p kc n", p=128))
    k.mark()
    wspf = k.sb("wspf", [128, 8, 128], F32)
    dma("sp", wspf, wspf.ap().rearrange("p a b -> p (a b)"), B_in, wsp_d.ap())
    tt("dve", wsp, wsp.ap(), wspf, wspf.ap(), cf, cf[:, C_TRIU:C_TRIU + 128].unsqueeze(1).to_broadcast([128, 8, 128]), ALU.mult)
    dma("pool", convw, convw.ap().rearrange("p a b -> p (a b)"), B_in, vecs_d[:, V_CONVW:V_CONVW + 6144], max_dma_last_dim=4096)
    dma("pool", convb, convb.ap(), B_in, vecs_d[:, V_CONVB:V_CONVB + 1536], max_dma_last_dim=4096)
    dma("sp", vsm, vsm.ap(), B_in, vecs_d[:, V_DTB:V_DTB + 48])
    dma("sp", ssdn, ssdn.ap(), B_in, vecs_d[:, V_SSDN:V_SSDN + 1024])
    dma("sp", gmlpn, gmlpn.ap(), B_in, vecs_d[:, V_GMLPN:V_GMLPN + 1024])
    dma("sp", bsp, bsp.ap(), B_in, bsp_d.ap())
    act(aneg, aneg.ap(), vsm, vsm[:, 16:32], AF.Exp)
    ts("dve", aneg, aneg.ap(), aneg, aneg.ap(), -1.0, None, ALU.mult)
    k.barrier()
    k.release()
    dtb = vsm[:, 0:16]
    dsk = vsm[:, 32:48]

    pj = k.sb("pj", [128, IN_DIM], BF16)
    xbcs = [k.sb("xbc%d" % i, [128, 1536], BF16) for i in range(2)]
    dtall = k.sb("dtall", [128, NCH, 16], F32)
    xt = k.sb("xt2", [128, 1024], F32)
    carry = k.sb("carry", [128, 1536], BF16)
    xsk = k.sb("xsk", [128, 4, 1536], BF16)
    xa = k.sb("xa", [128, 1536], BF16)
    BCT = k.sb("BCT", [128, 4, 128], BF16)
    s16 = k.sb("s16", [128, 8, 16], F32)
    acs2 = k.sb("acs2", [128, 32], F32)
    rAs = [k.sb("rA%d" % i, [128, 4, 128], F32) for i in range(2)]
    LT = k.sb("LT", [128, 16, 128], BF16)
    Mh = LT
    xdt = k.sb("xdt", [128, 1024], BF16)
    xdtd = k.sb("xdtd", [128, 1024], BF16)
    xsD = k.sb("xsD", [128, 1024], BF16)
    yf = k.sb("yf", [128, 1024], F32)
    t1 = k.sb("t1", [128, 1024], F32)
    stf = k.sb("stf", [128, 1024], F32)
    stb = k.sb("stb", [128, 1024], BF16)
    ybr = k.sb("ybr", [128, 1024], BF16)
    bT = k.sb("bT", [128, 8, 128], BF16)
    sg = k.sb("sg", [128, 1024], BF16)
    mg = k.sb("mg", [128, 1024], F32)
    sm8 = k.sb("sm8", [128, 16], F32)
    gu = xsD
    pexp = k.sb("pexp", [128, 1024], BF16)
    pexp3 = pexp.ap().rearrange("p (a b) -> p a b", a=4)

    def s16v(i):
        return s16[:, i, :]

    def transpose8(src_b, src_ap_fn, dst):
        for kc in range(8):
            trn(ps[7], psb[7][:, kc * 128:(kc + 1) * 128], src_b, src_ap_fn(kc), idb)
        cp("act", dst, dst.ap().rearrange("p a b -> p (a b)"), ps[7], psb[7][:, :])

    sg2 = k.sb("sg2", [128, 1024], BF16)
    bT2 = k.sb("bT2", [128, 8, 128], BF16)
    ybr2 = k.sb("ybr2", [128, 1024], BF16)

    def branch_proj(br, first, sg=sg, pre=False, src=None):
        src = ybr if src is None else src
        if not pre:
            act(sg, sg.ap(), pj, pj[:, O_G + br * 1024:O_G + (br + 1) * 1024], AF.Tanh, scale=0.5)
        transpose8(src, lambda kc: src[:, kc * 128:(kc + 1) * 128], bT)
        for h2 in range(2):
            pb = ps[5 + h2]
            sl = slice(h2 * 512, (h2 + 1) * 512)
            for kc in range(8):
                mm(pb, pb.ap(), bT, bT[:, kc, :], wbr, wbr[:, br * 8 + kc, sl], st=(kc == 0), sp=(kc == 7))
            if first:
                stt(mg, mg[:, sl], sg, sg[:, sl], 1.0, pb, pb.ap(), ALU.add, ALU.mult)
            else:
                stt(t1, t1[:, sl], sg, sg[:, sl], 1.0, pb, pb.ap(), ALU.add, ALU.mult)
                tt("pool", mg, mg[:, sl], mg, mg[:, sl], t1, t1[:, sl], ALU.add)

    k.op("sp", lambda e: e.dma_start(out=dtall[:, 0:n_ch, :], in_=dtraw_d[0:n_ch * 128, :].rearrange("(c p) h -> p c h", p=128)),
         reads=B_dtraw[0:n_ch], writes=[dtall], dma=True)
    tt("dve", dtall, dtall[:, 0:n_ch, :], dtall, dtall[:, 0:n_ch, :], vsm, dtb.unsqueeze(1).to_broadcast([128, n_ch, 16]), ALU.add)
    act(dtall, dtall[:, 0:n_ch, :], dtall, dtall[:, 0:n_ch, :], AF.Exp)
    act(dtall, dtall[:, 0:n_ch, :], dtall, dtall[:, 0:n_ch, :], AF.Ln, bias=1.0)

    def load_xbc(c):
        dma("sp", xbcs[c % 2], xbcs[c % 2].ap(), B_proj[c], proj_d[c * 128:(c + 1) * 128, O_XBC:O_XBC + 1536])

    xs3v = xa[:, 0:1024].rearrange("p (h q) -> p h q", h=16)

    def bc16(i):
        return s16v(i).unsqueeze(2).to_broadcast([128, 16, 64])

    def ssd_front(c):
        b, ci = c // 16, c % 16
        xbc = xbcs[c % 2]
        for kk in range(4):
            tt("dve" if kk < 3 else "pool", xsk, xsk[:, kk, :], xbc, xbc.ap(), convw, convw[:, kk, :], ALU.mult)
        yield
        for nb in range(3):
            pb = ps[nb]
            cs = slice(nb * 512, (nb + 1) * 512)
            mm(pb, pb.ap(), cbf, cbf[:, C_R0:C_R0 + 128], convb, convb[:, cs], st=True, sp=False)
            for kk in range(3):
                mm(pb, pb.ap(), cbf, cbf[:, C_S + kk * 128:C_S + (kk + 1) * 128], xsk, xsk[:, kk, cs], st=False, sp=False)
            if ci > 0:
                mm(pb, pb.ap(), cbf, idb, carry, carry[:, cs], st=False, sp=False)
            mm(pb, pb.ap(), cbf, idb, xsk, xsk[:, 3, cs], st=False, sp=True)
            if nb == 1:
                yield
        yield
        if ci < 15:
            for nb in range(3):
                pb = ps[3 + nb % 2]
                cs = slice(nb * 512, (nb + 1) * 512)
                for kk in range(3):
                    mm(pb, pb.ap(), cbf, cbf[:, C_P + kk * 128:C_P + (kk + 1) * 128], xsk, xsk[:, kk, cs], st=(kk == 0), sp=(kk == 2))
                cp("act", carry, carry[:, cs], pb, pb.ap())
        for nb in range(3):
            act(xa, xa[:, nb * 512:(nb + 1) * 512], ps[nb], ps[nb].ap(), AF.Silu)
        cp("dve", s16, s16v(2), dtall, dtall[:, c, :])
        tt("dve", s16, s16v(3), dtall, dtall[:, c, :], aneg, aneg.ap(), ALU.mult)
        yield
        for q in range(4):
            trn(ps[3], psb[3][:, q * 128:(q + 1) * 128], xa, xa[:, 1024 + q * 128:1024 + (q + 1) * 128], idb)
        cp("dve", BCT, BCT.ap().rearrange("p a b -> p (a b)"), ps[3], psb[3][:, 0:512])
        mm(ps[4], ps[4][:, 0:16], cf, cf[:, C_TRIU:C_TRIU + 128], s16, s16v(3))
        mm(ps[4], ps[4][:, 16:32], cf, cf[:, C_ONES:C_ONES + 128], s16, s16v(3))
        cp("dve", acs2, acs2.ap(), ps[4], ps[4][:, 0:32])
        ts("dve", s16, s16v(4), acs2, acs2[:, 0:16], -1.0, None, ALU.mult)
        act(s16, s16v(5), acs2, acs2[:, 0:16], AF.Exp)
        tt("dve", s16, s16v(6), acs2, acs2[:, 16:32], acs2, acs2[:, 0:16], ALU.subtract)
        act(s16, s16v(6), s16, s16v(6), AF.Exp)
        act(s16, s16v(7), acs2, acs2[:, 16:32], AF.Exp)
        for q in range(4):
            rA = rAs[q % 2]
            tt("pool", rA, rA.ap(), cf, cf[:, C_TRIU:C_TRIU + 128].unsqueeze(1).to_broadcast([128, 4, 128]),
               s16, s16[:, 3, 4 * q:4 * q + 4].unsqueeze(2).to_broadcast([128, 4, 128]), ALU.mult)
            pb = ps[q]
            mm(pb, pb.ap(), cf, cf[:, C_ONES:C_ONES + 128], rA, rA.ap().rearrange("p a b -> p (a b)"), st=True, sp=False)
            for hh in range(4):
                mm(pb, pb[:, hh * 128:(hh + 1) * 128], cbf, idb, cbf, cbf[:, C_NEG:C_NEG + 128], st=False, sp=(hh == 3))
            for hh in range(4):
                h = 4 * q + hh
                act(LT, LT[:, h, :], pb, pb[:, hh * 128:(hh + 1) * 128], AF.Exp, bias=s16[:, 4, h:h + 1], extra=[s16])
        for g in range(2):
            mm(ps[4], ps[4][:, g * 128:(g + 1) * 128], BCT, BCT[:, g, :], BCT, BCT[:, 2 + g, :])
        for g in range(2):
            tt("dve", Mh, Mh[:, g * 8:(g + 1) * 8, :], ps[4], ps[4][:, g * 128:(g + 1) * 128].unsqueeze(1).to_broadcast([128, 8, 128]),
               LT, LT[:, g * 8:(g + 1) * 8, :], ALU.mult)
        yield
        tt("dve", xdt, xdt.ap().rearrange("p (h q) -> p h q", h=16), xa, xs3v, s16, bc16(2), ALU.mult)
        tt("dve", xsD, xsD.ap().rearrange("p (h q) -> p h q", h=16), xa, xs3v, vsm, dsk.unsqueeze(2).to_broadcast([128, 16, 64]), ALU.mult)
        if ci < 15:
            tt("pool", xdtd, xdtd.ap().rearrange("p (h q) -> p h q", h=16), xdt, xdt.ap().rearrange("p (h q) -> p h q", h=16),
               s16, bc16(6), ALU.mult)
        yield

    def back_head(c):
        b, ci = c // 16, c % 16
        if c == 0:
            dma("sp", pj, pj.ap(), B_proj[c], proj_d[c * 128:(c + 1) * 128, :])
        dma("sp", xt, xt.ap(), B_in, x_d[c * 128:(c + 1) * 128, :])
        if c + 1 < n_ch:
            load_xbc(c + 1)
        act(sg, sg.ap(), pj, pj[:, O_G:O_G + 1024], AF.Tanh, scale=0.5)
        act(sg2, sg2.ap(), pj, pj[:, O_G + 2048:O_G + 3072], AF.Tanh, scale=0.5)
        for g in range(2):
            pb = ps[g]
            mm(pb, pb.ap(), cbf, idb, xsD, xsD[:, g * 512:(g + 1) * 512], st=True, sp=False)
            for hh in range(8):
                h = g * 8 + hh
                mm(pb, pb[:, hh * 64:(hh + 1) * 64], Mh, Mh[:, h, :], xdt, xdt[:, h * 64:(h + 1) * 64], st=False, sp=(hh == 7))
        if ci > 0:
            for g in range(2):
                pb = ps[2 + g]
                sl = slice(g * 512, (g + 1) * 512)
                mm(pb, pb.ap(), BCT, BCT[:, 2 + g, :], stb, stb[:, sl])
                tt("dve", t1, t1[:, sl].rearrange("p (h q) -> p h q", h=8), pb, pb.ap().rearrange("p (h q) -> p h q", h=8),
                   s16, s16[:, 5, g * 8:(g + 1) * 8].unsqueeze(2).to_broadcast([128, 8, 64]), ALU.mult)
                tt("dve", yf, yf[:, sl], ps[g], ps[g].ap(), t1, t1[:, sl], ALU.add)
        else:
            for g in range(2):
                cp("dve", yf, yf[:, g * 512:(g + 1) * 512], ps[g], ps[g].ap())
        if ci < 15:
            for g in range(2):
                pb = ps[4 + g]
                sl = slice(g * 512, (g + 1) * 512)
                mm(pb, pb.ap(), xa, xa[:, 1024 + g * 128:1024 + (g + 1) * 128], xdtd, xdtd[:, sl])
                if ci > 0:
                    tt("pool", stf, stf[:, sl].rearrange("p (h q) -> p h q", h=8), stf, stf[:, sl].rearrange("p (h q) -> p h q", h=8),
                       s16, s16[:, 7, g * 8:(g + 1) * 8].unsqueeze(2).to_broadcast([128, 8, 64]), ALU.mult)
                    tt("dve", stf, stf[:, sl], pb, pb.ap(), stf, stf[:, sl], ALU.add)
                else:
                    cp("dve", stf, stf[:, sl], pb, pb.ap())
                cp("act", stb, stb[:, sl], stf, stf[:, sl])
        act(t1, t1.ap(), pj, pj[:, O_Z:O_Z + 1024], AF.Silu)
        tt("dve", yf, yf.ap(), yf, yf.ap(), t1, t1.ap(), ALU.mult)
        rmsnorm(yf, yf.ap(), ssdn, ssdn.ap(), ybr, ybr.ap(), "ssd", pool_pow=True)
        xattn(c)
        branch_proj(0, True, pre=True)
        act(gu, gu.ap(), pj, pj[:, O_U:O_U + 1024], AF.Gelu_apprx_tanh)
        act(yf, yf.ap(), pj, pj[:, O_V:O_V + 1024], AF.Gelu_apprx_tanh)
        rmsnorm(yf, yf.ap(), gmlpn, gmlpn.ap(), xdt, xdt.ap(), "gmlp", pool_pow=True)
        for g in range(8):
            pb = ps[g // 4]
            mm(pb, pb[:, (g % 4) * 128:(g % 4 + 1) * 128], wsp, wsp[:, g, :], xdt, xdt[:, g * 128:(g + 1) * 128])
        for h2 in range(2):
            pb = ps[h2]
            sl = slice(h2 * 512, (h2 + 1) * 512)
            tt("dve", t1, t1[:, sl].rearrange("p (g q) -> p g q", g=4), pb, pb.ap().rearrange("p (g q) -> p g q", g=4),
               bsp, bsp[:, h2 * 4:(h2 + 1) * 4].unsqueeze(2).to_broadcast([128, 4, 128]), ALU.add)
            tt("pool", ybr, ybr[:, sl], t1, t1[:, sl], gu, gu[:, sl], ALU.mult)
        branch_proj(1, False)
        if c + 1 < n_ch:
            dma("sp", pj, pj.ap(), B_proj[c + 1], proj_d[(c + 1) * 128:(c + 2) * 128, :])

    def xattn(c):
        b = c // 16
        transpose8(pj, lambda kc: pj[:, O_Q + kc * 128:O_Q + (kc + 1) * 128], bT2)
        for hd in range(4):
            pb = ps[5 + hd // 2]
            for j in range(2):
                mm(pb, pb[:, (hd % 2) * 256:(hd % 2 + 1) * 256], bT2, bT2[:, 2 * hd + j, :], KT, KT[:, b, 2 * hd + j, :], st=(j == 0), sp=(j == 1))
        for h2 in range(2):
            k.op("dve", lambda e, h2=h2: e.tensor_reduce(out=sm8[:, 2 * h2:2 * h2 + 2], in_=ps[5 + h2].ap().rearrange("p (a b) -> p a b", a=2),
                                                          axis=AX.X, op=ALU.max), reads=[ps[5 + h2]], writes=[sm8])
        ts("dve", sm8, sm8[:, 4:8], sm8, sm8[:, 0:4], -1.0 / 16.0, None, ALU.mult)
        for hd in range(4):
            pb = ps[5 + hd // 2]
            act(pexp, pexp3[:, hd, :], pb, pb[:, (hd % 2) * 256:(hd % 2 + 1) * 256], AF.Exp, bias=sm8[:, 4 + hd:5 + hd],
                scale=1.0 / 16.0, accum=sm8[:, 8 + hd:9 + hd], extra=[sm8], wextra=[sm8])
        k.op("dve", lambda e: e.reciprocal(out=sm8[:, 12:16], in_=sm8[:, 8:12]), reads=[sm8], writes=[sm8])
        transpose8(pexp, lambda kc: pexp3[:, kc // 2, (kc % 2) * 128:(kc % 2 + 1) * 128], bT2)
        for hd in range(4):
            pb = ps[5 + hd // 2]
            for j in range(2):
                mm(pb, pb[:, (hd % 2) * 256:(hd % 2 + 1) * 256], bT2, bT2[:, 2 * hd + j, :], Vm, Vm[:, b, j, hd * 256:(hd + 1) * 256], st=(j == 0), sp=(j == 1))
        for h2 in range(2):
            pb = ps[5 + h2]
            tt("dve", ybr2, ybr2[:, h2 * 512:(h2 + 1) * 512].rearrange("p (a q) -> p a q", a=2), pb, pb.ap().rearrange("p (a q) -> p a q", a=2),
               sm8, sm8[:, 12 + 2 * h2:14 + 2 * h2].unsqueeze(2).to_broadcast([128, 2, 256]), ALU.mult)

    def tail(c):
        b, ci = c // 16, c % 16
        branch_proj(2, False, sg=sg2, pre=True, src=ybr2)
        yield
        act(ybr, ybr.ap(), mg, mg.ap(), AF.Copy, scale=0.5)
        transpose8(ybr, lambda kc: ybr[:, kc * 128:(kc + 1) * 128], bT)
        yield
        for h2 in range(2):
            pb = ps[5 + h2]
            sl = slice(h2 * 512, (h2 + 1) * 512)
            for kc in range(8):
                mm(pb, pb.ap(), bT, bT[:, kc, :], wout, wout[:, kc, sl], st=(kc == 0), sp=(kc == 7))
            tt("dve", xt, xt[:, sl], pb, pb.ap(), xt, xt[:, sl], ALU.add)
        dma("sp", B_x1[c], x1_d[c * 128:(c + 1) * 128, :], xt, xt.ap())
        yield

    def interleave(*gens):
        gens = [g for g in gens if g is not None]
        while gens:
            for g in list(gens):
                try:
                    next(g)
                except StopIteration:
                    gens.remove(g)

    load_xbc(0)
    interleave(ssd_front(0))
    for c in range(n_ch):
        back_head(c)
        interleave(ssd_front(c + 1) if c + 1 < n_ch else None, tail(c))
    k.barrier()
    k.release()
    k.sb_hi = SB_HI
    if stop_after == 2:
        k.emit()
        return nc

    ITt = k.sb("ITt", [128, TOK], BF16)
    JTt = k.sb("JTt", [128, TOK], BF16)
    GTt = k.sb("GTt", [128, TOK], BF16)
    k.mark()
    wpq = k.sb("wpq", [128, 8, 2048], BF16)
    keysb = k.sb("keysb", [128, 16, 128], BF16)
    lnffn = k.sb("lnffn", [128, 1024], F32)
    xts = [k.sb("xt3_%d" % i, [128, 1024], F32) for i in range(2)]
    xnb = k.sb("xnb", [128, 1024], BF16)
    xnTc = [k.sb("xnTc%d" % i, [128, 8, 128], BF16) for i in range(2)]
    qpT = k.sb("qpT", [128, 16, 128], BF16)
    sc = k.sb("sc", [128, 16, 128], F32)
    scw = k.sb("scw", [128, 16, 128], F32)
    tv = k.sb("tv", [128, 16, 16], F32)
    ti = k.sb("ti", [128, 16, 16], U32)
    tif = k.sb("tif", [128, 16, 16], F32)
    cand = k.sb("cand", [128, 8, 256], F32)
    candw = k.sb("candw", [128, 8, 256], F32)
    bv = k.sb("bv", [128, 8, 16], F32)
    bpi = k.sb("bpi", [128, 8, 16], U32)
    bpa = k.sb("bpa", [128, 8, 16], U32)
    bpb = k.sb("bpb", [128, 8, 16], U32)
    bpf = k.sb("bpf", [128, 2, 128], F32)
    eq = cand
    eq4 = cand.ap().rearrange("p h (a b) -> p h a b", a=16)
    IJ = k.sb("IJ", [128, 3, 128], F32)
    IJb = k.sb("IJb", [128, 3, 128], BF16)
    gs = k.sb("gs", [128, 16], F32)
    dma("sp", wpq, wpq.ap(), B_wpqb, wpqb_d.ap().rearrange("(kc p) n -> p kc n", p=128))
    dma("pool", keysb, keysb.ap().rearrange("p a b -> p (a b)"), B_in, keys_d.ap(), max_dma_last_dim=4096)
    dma("sp", lnffn, lnffn.ap(), B_in, vecs_d[:, V_LNFFN:V_LNFFN + 1024])
    iota16 = cf[:, 384:400]
    tv_s = [tv.sub("g%d" % i) for i in range(16)]
    ti_s = [ti.sub("g%d" % i) for i in range(16)]
    scw_s = [scw.sub("g%d" % i) for i in range(16)]
    bv_s = [bv.sub("h%d" % i) for i in range(8)]
    bpi_s = [bpi.sub("h%d" % i) for i in range(8)]
    candw_s = [candw.sub("h%d" % i) for i in range(8)]
    scs = [sc, k.sb("sc_b", [128, 16, 128], F32)]
    xs3 = k.sb("xs3", [128, 1024], F32)
    sm3 = k.sb("sm3", [128, 8], F32)
    k.op("pool", lambda e: e.memset(sm3[:, 4:5], -0.5), writes=[sm3])

    def front(c):
        xt = xts[c % 2]
        xc = xnTc[c % 2]
        sc_ = scs[c % 2]
        dma("sp", xt, xt.ap(), B_x1[c], x1_d[c * 128:(c + 1) * 128, :])
        act(junk, junk.ap(), xt, xt.ap(), AF.Square, accum=sm3[:, 0:1], wextra=[sm3])
        ts("pool", sm3, sm3[:, 1:2], sm3, sm3[:, 0:1], 1.0 / D, EPS, ALU.mult, ALU.add)
        k.op("pool", lambda e: e.tensor_tensor(out=sm3[:, 2:3], in0=sm3[:, 1:2], in1=sm3[:, 4:5], op=ALU.pow), reads=[sm3], writes=[sm3])
        act(xs3, xs3.ap(), xt, xt.ap(), AF.Copy, scale=sm3[:, 2:3], extra=[sm3])
        tt("pool", xnb, xnb.ap(), xs3, xs3.ap(), lnffn, lnffn.ap(), ALU.mult)
        for kc in range(8):
            trn(ps[0], psb[0][:, kc * 128:(kc + 1) * 128], xnb, xnb[:, kc * 128:(kc + 1) * 128], idb)
        cp("act", xc, xc.ap().rearrange("p a b -> p (a b)"), ps[0], psb[0][:, :])
        dma("sp", B_xng[c], xng_d[c // 2, :, :, (c % 2) * 128:(c % 2 + 1) * 128], xc, xc.ap())
        for cc in range(16):
            pb = ps[1 + cc // 4]
            for kc in range(8):
                mm(pb, pb[:, (cc % 4) * 128:(cc % 4 + 1) * 128], wpq, wpq[:, kc, cc * 128:(cc + 1) * 128],
                   xc, xc[:, kc, :], st=(kc == 0), sp=(kc == 7))
        for q in range(4):
            cp("act", qpT, qpT[:, 4 * q:4 * q + 4, :].rearrange("p a b -> p (a b)"), ps[1 + q], ps[1 + q].ap())
        for cc in range(16):
            pb = ps[(5 + cc // 4) % 8] if cc < 12 else ps[1]
            mm(pb, pb[:, (cc % 4) * 128:(cc % 4 + 1) * 128], qpT, qpT[:, cc, :], keysb, keysb[:, cc, :])
        for q in range(4):
            pb = ps[(5 + q) % 8] if q < 3 else ps[1]
            cp("act", sc_, sc_[:, 4 * q:4 * q + 4, :].rearrange("p a b -> p (a b)"), pb, pb.ap())

    front(0)
    for c in range(n_ch):
        if c + 1 < n_ch:
            front(c + 1)
        sc = scs[c % 2]
        for cc in range(16):
            k.op("dve", lambda e, cc=cc, sc=sc: e.max(out=tv[:, cc, 0:8], in_=sc[:, cc, :]), reads=[sc], writes=[tv_s[cc]])
        for cc in range(16):
            k.op("dve", lambda e, cc=cc, sc=sc: e.max_index(out=ti[:, cc, 0:8], in_max=tv[:, cc, 0:8], in_values=sc[:, cc, :]), reads=[sc, tv_s[cc]], writes=[ti_s[cc]])
        for cc in range(16):
            k.op("dve", lambda e, cc=cc, sc=sc: e.match_replace(out=scw[:, cc, :], in_to_replace=tv[:, cc, 0:8], in_values=sc[:, cc, :], imm_value=-1e30),
                 reads=[sc, tv_s[cc]], writes=[scw_s[cc]])
        for cc in range(16):
            k.op("dve", lambda e, cc=cc, sc=sc: e.max(out=tv[:, cc, 8:16], in_=scw[:, cc, :]), reads=[scw_s[cc]], writes=[tv_s[cc]])
        for cc in range(16):
            k.op("dve", lambda e, cc=cc, sc=sc: e.max_index(out=ti[:, cc, 8:16], in_max=tv[:, cc, 8:16], in_values=scw[:, cc, :]), reads=[scw_s[cc], tv_s[cc]], writes=[ti_s[cc]])
        k.op("dve", lambda e: e.tensor_copy(out=tif.ap(), in_=ti.ap()), reads=ti_s, writes=[tif])
        tv4 = tv.ap().rearrange("p (h i) a -> p h i a", i=2)
        k.op("dve", lambda e, tv4=tv4: e.tensor_tensor(out=cand.ap().rearrange("p h (a b) -> p h a b", a=16),
                                                    in0=tv4[:, :, 0, :].unsqueeze(3).to_broadcast([128, 8, 16, 16]),
                                                    in1=tv4[:, :, 1, :].unsqueeze(2).to_broadcast([128, 8, 16, 16]), op=ALU.add),
             reads=tv_s, writes=[cand])
        for h in range(8):
            k.op("dve", lambda e, h=h: e.max(out=bv[:, h, 0:8], in_=cand[:, h, :]), reads=[cand], writes=[bv_s[h]])
        for h in range(8):
            k.op("dve", lambda e, h=h: e.max_index(out=bpi[:, h, 0:8], in_max=bv[:, h, 0:8], in_values=cand[:, h, :]), reads=[cand, bv_s[h]], writes=[bpi_s[h]])
        for h in range(8):
            k.op("dve", lambda e, h=h: e.match_replace(out=candw[:, h, :], in_to_replace=bv[:, h, 0:8], in_values=cand[:, h, :], imm_value=-1e30),
                 reads=[cand, bv_s[h]], writes=[candw_s[h]])
        for h in range(8):
            k.op("dve", lambda e, h=h: e.max(out=bv[:, h, 8:16], in_=candw[:, h, :]), reads=[candw_s[h]], writes=[bv_s[h]])
        for h in range(8):
            k.op("dve", lambda e, h=h: e.max_index(out=bpi[:, h, 8:16], in_max=bv[:, h, 8:16], in_values=candw[:, h, :]), reads=[candw_s[h], bv_s[h]], writes=[bpi_s[h]])
        k.op("dve", lambda e: e.tensor_scalar(out=bpa.ap(), in0=bpi.ap(), scalar1=4, scalar2=None, op0=ALU.logical_shift_right), reads=bpi_s, writes=[bpa])
        k.op("dve", lambda e: e.tensor_scalar(out=bpb.ap(), in0=bpi.ap(), scalar1=15, scalar2=None, op0=ALU.bitwise_and), reads=bpi_s, writes=[bpb])
        cp("dve", bpf, bpf[:, 0, :], bpa, bpa.ap().rearrange("p a b -> p (a b)"))
        cp("dve", bpf, bpf[:, 1, :], bpb, bpb.ap().rearrange("p a b -> p (a b)"))
        tif4 = tif.ap().rearrange("p (h i) a -> p h i a", i=2)
        for w_ in range(2):
            pos = bpf[:, w_, :].rearrange("p (h k) -> p h k", h=8)
            tt("dve", eq, eq4, bpf, pos.unsqueeze(3).to_broadcast([128, 8, 16, 16]),
               cf, iota16.unsqueeze(1).unsqueeze(1).to_broadcast([128, 8, 16, 16]), ALU.is_equal)
            tt("dve", eq, eq4, eq, eq4, tif, tif4[:, :, w_, :].unsqueeze(2).to_broadcast([128, 8, 16, 16]), ALU.mult)
            k.op("dve", lambda e, w_=w_: e.tensor_reduce(out=IJ[:, w_, :], in_=cand.ap().rearrange("p h (k a) -> p (h k) a", a=16), axis=AX.X, op=ALU.add),
                 reads=[eq], writes=[IJ])
        g3 = IJ[:, 2, :].rearrange("p (h k) -> p h k", h=8)
        k.op("dve", lambda e, g3=g3: e.tensor_tensor(out=g3, in0=bv.ap(), in1=bv[:, :, 0:1].to_broadcast([128, 8, 16]), op=ALU.subtract),
             reads=bv_s, writes=[IJ])
        act(IJ, IJ[:, 2, :], IJ, IJ[:, 2, :], AF.Exp)
        k.op("dve", lambda e: e.tensor_reduce(out=gs[:, 0:8], in_=g3, axis=AX.X, op=ALU.add), reads=[IJ], writes=[gs])
        k.op("dve", lambda e: e.reciprocal(out=gs[:, 8:16], in_=gs[:, 0:8]), reads=[gs], writes=[gs])
        tt("dve", IJ, g3, IJ, g3, gs, gs[:, 8:16].unsqueeze(2).to_broadcast([128, 8, 16]), ALU.mult)
        cp("dve", IJb, IJb.ap(), IJ, IJ.ap())
        for w_ in range(3):
            trn(ps[0], psb[0][:, w_ * 128:(w_ + 1) * 128], IJb, IJb[:, w_, :], idb)
        for w_, dst in enumerate((ITt, JTt, GTt)):
            cp("act", dst, dst[:, c * 128:(c + 1) * 128], ps[0], psb[0][:, w_ * 128:(w_ + 1) * 128])
    k.barrier()
    k.release()
    if stop_after == 3:
        dump("ITt", ITt, ITt[:, 0:ntok], [128, ntok])
        dump("JTt", JTt, JTt[:, 0:ntok], [128, ntok])
        dump("GTt", GTt, GTt[:, 0:ntok], [128, ntok])
        k.emit()
        return nc

    k.mark()
    TG = 256
    Gs = [k.sb("Gs%d" % i, [128, TG, 128], BF16) for i in range(2)]
    xg = [k.sb("xg%d" % i, [128, 8, TG], BF16) for i in range(2)]
    lnfin = k.sb("lnfin", [128, 1024], F32)
    iob = cbf[:, C_IOTA:C_IOTA + 128]
    NB4 = 5
    UT = [k.sb("UT%d" % i, [128, 8, 128], BF16) for i in range(NB4)]
    Vi = [k.sb("Vi%d" % i, [128, 1024], BF16) for i in range(NB4)]
    Rt = [k.sb("Rt%d" % i, [128, 128], BF16) for i in range(4)]
    Ct = [k.sb("Ct%d" % i, [128, 128], BF16) for i in range(4)]
    ga = [k.sb("ga%d" % i, [128, TG], BF16) for i in range(2)]
    Hh = [k.sb("Hh%d" % i, [128, TG], BF16) for i in range(3)]
    xt4 = [k.sb("xt4_%d" % i, [128, 1024], F32) for i in range(2)]
    yo = [k.sb("yo%d" % i, [128, 1024], F32) for i in range(1)]
    dma("pool", lnfin, lnfin.ap(), B_in, vecs_d[:, V_LNFIN:V_LNFIN + 1024])
    ngrp = ntok // TG

    def load_xg(gq):
        dst = xg[gq % 2]
        k.op("pool", lambda e: e.dma_start(out=dst.ap(), in_=xng_d[gq]), reads=[B_xng[2 * gq], B_xng[2 * gq + 1]], writes=[dst], dma=True)

    def g_tokens(gq, tl, defer=None):
        G_ = Gs[gq % 2]
        for tloc in tl:
            s_, tq = tloc % 4, tloc // 4
            pb = ps[7]
            t = gq * TG + tloc
            r_, c_ = Rt[s_], Ct[s_]
            ts("dve", r_, r_.ap(), cbf, iob, ITt[:, t:t + 1], GTt[:, t:t + 1], ALU.is_equal, ALU.mult, extra=[ITt, GTt])
            ts("dve", c_, c_.ap(), cbf, iob, JTt[:, t:t + 1], None, ALU.is_equal, extra=[JTt])
            mm(pb, pb[:, s_ * 128:(s_ + 1) * 128], c_, c_.ap(), r_, r_.ap())
            if s_ == 3:
                if defer is None:
                    cp("act", G_, G_[:, tq * 4:tq * 4 + 4, :].rearrange("p t i -> p (t i)"), pb, pb.ap())
                else:
                    defer.append((G_, G_[:, tq * 4:tq * 4 + 4, :].rearrange("p t i -> p (t i)"), pb))

    def issue_loads(gq, i):
        u_, v_ = UT[i % NB4], Vi[i % NB4]
        dma("sp", u_, u_.ap().rearrange("p a b -> p (a b)"), B_ub[i // 8], ub_d[i * 128:(i + 1) * 128, :])
        dma("sp", v_, v_.ap(), B_vb[i // 8], vb_d[i * 128:(i + 1) * 128, :])

    def a_mm(gq, i):
        u_ = UT[i % NB4]
        pa = ps[4 + i % 3]
        x_ = xg[gq % 2]
        for kc in range(8):
            mm(pa, pa[:, 0:TG], u_, u_[:, kc, :], x_, x_[:, kc, :], st=(kc == 0), sp=(kc == 7))

    def gelu_mult(gq, i):
        pa = ps[4 + i % 3]
        g_, h_ = ga[i % 2], Hh[i % 3]
        act(g_, g_.ap(), pa, pa[:, 0:TG], AF.Gelu_apprx_tanh)
        tt("dve", h_, h_.ap(), g_, g_.ap(), Gs[gq % 2], Gs[gq % 2][:, :, i], ALU.mult)

    load_xg(0)
    g_tokens(0, range(TG))
    pending_fin = []

    def make_fin(t0):
        def fin():
            for tb in range(TG // 128):
                cidx = (t0 // 128) + tb
                xt, y_ = xt4[tb % 2], yo[0]
                rmsnorm(xt, xt.ap(), lnfin, lnfin.ap(), y_, y_.ap(), "fin", pool_pow=True)
                dma("pool", B_out, out_d[cidx * 128:(cidx + 1) * 128, :], y_, y_.ap())
        return fin

    for gq in range(ngrp):
        t0 = gq * TG
        if gq + 1 < ngrp:
            load_xg(gq + 1)
        issue_loads(gq, 0)
        a_mm(gq, 0)
        issue_loads(gq, 1)
        a_mm(gq, 1)
        gelu_mult(gq, 0)
        for i in range(128):
            if i + 2 < 128:
                issue_loads(gq, i + 2)
                a_mm(gq, i + 2)
            dfr = []
            if gq + 1 < ngrp:
                g_tokens(gq + 1, [2 * i, 2 * i + 1], dfr)
            if i + 1 < 128:
                gelu_mult(gq, i + 1)
            for (gb_, gap_, pb_) in dfr:
                cp("act", gb_, gap_, pb_, pb_.ap())
            v_ = Vi[i % NB4]
            h_ = Hh[i % 3]
            for tb in range(TG // 128):
                for dh in range(2):
                    pb = ps[tb * 2 + dh]
                    mm(pb, pb.ap(), h_, h_[:, tb * 128:(tb + 1) * 128], v_, v_[:, dh * 512:(dh + 1) * 512], st=(i == 0), sp=(i == 127))
            if i == 3:
                while pending_fin:
                    pending_fin.pop(0)()
                for tb in range(TG // 128):
                    cidx = (t0 // 128) + tb
                    dma("pool", xt4[tb % 2], xt4[tb % 2].ap(), B_x1[cidx], x1_d[cidx * 128:(cidx + 1) * 128, :])
        for tb in range(TG // 128):
            xt = xt4[tb % 2]
            for dh in range(2):
                pb = ps[tb * 2 + dh]
                tt("dve", xt, xt[:, dh * 512:(dh + 1) * 512], pb, pb.ap(), xt, xt[:, dh * 512:(dh + 1) * 512], ALU.add)
        pending_fin.append(make_fin(t0))
    while pending_fin:
        pending_fin.pop(0)()
    k.op("pool", None, reads=[B_out])
    k.barrier()
    k.release()
    k.emit()
    return nc


def host_consts():
    c = np.zeros((128, NC_CONST), np.float32)
    r = np.arange(128)
    c[:, C_ID:C_ID + 128] = np.eye(128)
    c[:, C_TRIU:C_TRIU + 128] = (r[:, None] <= r[None, :])
    c[:, C_ONES:C_ONES + 128] = 1.0
    for kk in range(3):
        c[:, C_S + kk * 128:C_S + (kk + 1) * 128] = (r[None, :] == r[:, None] + (3 - kk))
        c[:, C_P + kk * 128:C_P + (kk + 1) * 128] = (r[None, :] == r[:, None] + (3 - kk) - 128)
    c[:, C_NEG:C_NEG + 128] = np.where(r[None, :] < r[:, None], -30000.0, 0.0)
    c[:, C_IOTA:C_IOTA + 128] = r[None, :]
    c[0, C_R0:C_R0 + 128] = 1.0
    return c


def prep_inputs(x, mem, ln_mix, w_in, conv_w, conv_b, dt_bias, a_log, d_skip, ssd_norm, gmlp_norm, w_spatial,
                b_spatial, ln_mem, w_mem_kv, w_branch, w_out, ln_ffn, w_peer_q, peer_keys, peer_u, peer_v, ln_final):
    f = lambda a: np.ascontiguousarray(np.asarray(a, dtype=np.float32))
    vec = np.concatenate([f(ln_mix[0]), f(conv_w[0]).reshape(-1), f(conv_b[0]), f(dt_bias[0]), f(a_log[0]), f(d_skip[0]),
                          f(ssd_norm[0]), f(gmlp_norm[0]), f(ln_mem[0]), f(ln_ffn[0]), f(ln_final)])
    assert vec.shape[0] == NV
    vecs = np.ascontiguousarray(np.broadcast_to(vec[None, :], (128, NV)))
    keys = f(peer_keys[0])
    keysT = np.ascontiguousarray(keys.transpose(3, 0, 1, 2).reshape(128, 2048))
    wsp = f(w_spatial[0])
    wspT = np.ascontiguousarray(wsp.transpose(2, 0, 1).reshape(128, 1024))
    U = f(peer_u[0])
    Uh = np.ascontiguousarray(U.reshape(128, 128, 8, 128).transpose(0, 3, 2, 1).reshape(16384, 1024))
    shared = {
        "w_in": f(w_in[0]), "w_kv": f(w_mem_kv[0]), "w_br": f(w_branch[0]).reshape(3 * D, D), "w_out": f(w_out[0]),
        "w_pq": f(w_peer_q[0]), "keysT": keysT, "wspT": wspT, "Uh": Uh, "V": f(peer_v[0]), "vecs": vecs,
        "consts": host_consts(), "bsp": np.ascontiguousarray(f(b_spatial[0]).T),
    }
    xs = f(x).reshape(NCORES, TOK, D)
    ms = f(mem).reshape(NCORES, 512, D)
    in_maps = []
    for i in range(NCORES):
        m = dict(shared)
        m["x"] = xs[i]
        m["mem"] = ms[i]
        in_maps.append(m)
    return in_maps


def kernel(**inputs):
    in_maps = prep_inputs(**inputs)
    nc = build_nc()
    res = run_bass_kernel_spmd(nc, in_maps, core_ids=list(range(NCORES)))
    out = np.stack([np.asarray(r["out"], dtype=np.float32) for r in res.results], axis=0)
    return out.reshape(16, 2048, D)
```
